# Optimizing a Trainium2 kernel written in Bass

```python
import math
import jax
import jax.numpy as jnp
from jax import lax
import numpy as np


D_MODEL = 1024
BATCH = 4
SEQ = 4096
DEPTH = 4

GRID_W = 64
CTX_LEN = 256
EPS = 1e-6
F32 = jnp.float32

HEAD_DIM = 64
ROPE_THETA = 10000.0
ROPE_PAIRS = HEAD_DIM // 4

DA_HEADS = 4
DA_VDIM = 2 * HEAD_DIM
DA_QBLOCK = 128

WG_HEADS = 8
WG_KV_HEADS = 2
WG_GROUP = WG_HEADS // WG_KV_HEADS
WINDOW = 128
WG_BLOCK = 128

GD_HEADS = 4
GD_DK = 128
GD_DV = 128
GD_CONV = 5
GD_CHUNK = 64
GD_QKV_W = GD_HEADS * (2 * GD_DK + GD_DV)

N_BRANCH = 3
BRANCH_W = DA_HEADS * DA_VDIM

SPLIT_SIZES = (
    DA_HEADS * 2 * HEAD_DIM,
    DA_HEADS * 2 * HEAD_DIM,
    DA_HEADS * DA_VDIM,
    WG_HEADS * HEAD_DIM,
    WG_KV_HEADS * HEAD_DIM,
    WG_KV_HEADS * HEAD_DIM,
    GD_QKV_W,
    GD_HEADS * GD_DV,
    2 * GD_HEADS,
    2 * GD_HEADS,
    N_BRANCH * D_MODEL,
)
IN_COLS = sum(SPLIT_SIZES)

PEER_HEADS = 8
PEER_DQ = 256
PEER_HALF = PEER_DQ // 2
N_KEYS = 128
N_EXPERTS = N_KEYS * N_KEYS
PEER_TOPK = 16
PEER_BLOCK = 128

kernel_name = 'hybrid_diffusion_gated_branches_peer'


def rms_norm(x, g):
    xf = x.astype(F32)
    y = xf * lax.rsqrt(jnp.mean(xf * xf, axis=-1, keepdims=True) + EPS)
    return (y * g.astype(F32)).astype(x.dtype)


def l2_norm(x):
    return x * lax.rsqrt(jnp.sum(x * x, axis=-1, keepdims=True) + EPS)


def rope_2d_tables(rows):
    row = jnp.repeat(jnp.arange(rows), GRID_W).astype(F32)
    col = jnp.tile(jnp.arange(GRID_W), rows).astype(F32)
    inv = jnp.power(ROPE_THETA, -jnp.arange(ROPE_PAIRS, dtype=F32) / ROPE_PAIRS)
    ar = row[:, None] * inv
    ac = col[:, None] * inv
    ang = jnp.concatenate([ar, ar, ac, ac], axis=-1)
    return jnp.cos(ang), jnp.sin(ang)


def apply_rope(x, cos, sin):
    shp = (1, cos.shape[0]) + (1,) * (x.ndim - 3) + (HEAD_DIM,)
    xr = x.reshape(x.shape[:-1] + (2, 2, ROPE_PAIRS))
    rot = jnp.stack([-xr[..., 1, :], xr[..., 0, :]], axis=-2).reshape(x.shape)
    return x * cos.reshape(shp).astype(x.dtype) + rot * sin.reshape(shp).astype(x.dtype)


def split_cols(p):
    idx = [int(v) for v in np.cumsum(SPLIT_SIZES)[:-1]]
    return jnp.split(p, idx, axis=-1)


def ada_mod(cvec, w, b):
    mod = jax.nn.silu(cvec) @ w + b
    return [t[:, None, :] for t in jnp.split(mod, 6, axis=-1)]


def modulate(x, g, shift, scale):
    return rms_norm(x, g) * (1.0 + scale) + shift


def diff_core(q, k, v, lam):
    s = jnp.einsum('bqhmd,bkhmd->bhmqk', q, k).astype(F32) * (HEAD_DIM ** -0.5)
    p = jax.nn.softmax(s, axis=-1)
    w = (p[:, :, 0] - lam * p[:, :, 1]).astype(v.dtype)
    return jnp.einsum('bhqk,bkhe->bqhe', w, v)


def diff_attention(q, k, v, qc, kc, vc, q_g, k_g, lam_vecs, subln_g, lam_init, cos, sin, need_ctx):
    bn, s_len, _ = q.shape
    l_len = kc.shape[1]
    heads = lambda t, n: t.reshape(bn, n, DA_HEADS, 2, HEAD_DIM)
    q = apply_rope(rms_norm(heads(q, s_len), q_g), cos, sin)
    k = apply_rope(rms_norm(heads(k, s_len), k_g), cos, sin)
    v = v.reshape(bn, s_len, DA_HEADS, DA_VDIM)
    kc = rms_norm(heads(kc, l_len), k_g)
    vc = vc.reshape(bn, l_len, DA_HEADS, DA_VDIM)
    lv = lam_vecs.astype(F32)
    lam = jnp.exp(jnp.sum(lv[0, 0] * lv[0, 1])) - jnp.exp(jnp.sum(lv[1, 0] * lv[1, 1])) + lam_init
    k_all = jnp.concatenate([kc, k], axis=1)
    v_all = jnp.concatenate([vc, v], axis=1)
    nb = s_len // DA_QBLOCK
    qb = jnp.moveaxis(q.reshape(bn, nb, DA_QBLOCK, DA_HEADS, 2, HEAD_DIM), 1, 0)
    o = lax.map(lambda qq: diff_core(qq, k_all, v_all, lam), qb)
    o = jnp.moveaxis(o, 0, 1).reshape(bn, s_len, DA_HEADS, DA_VDIM)

    def post(t):
        return (rms_norm(t, subln_g) * (1.0 - lam_init)).reshape(t.shape[0], t.shape[1], BRANCH_W)

    out = post(o)
    out_c = post(diff_core(rms_norm(heads(qc, l_len), q_g), kc, vc, lam)) if need_ctx else None
    return out, out_c


def window_gqa(q, k, v, qc, kc, vc, q_g, k_g, sink, cos, sin, need_ctx):
    bn, s_len, _ = q.shape
    l_len = kc.shape[1]
    nb = s_len // WG_BLOCK
    scale = HEAD_DIM ** -0.5
    q = apply_rope(rms_norm(q.reshape(bn, s_len, WG_KV_HEADS, WG_GROUP, HEAD_DIM), q_g), cos, sin)
    k = apply_rope(rms_norm(k.reshape(bn, s_len, WG_KV_HEADS, HEAD_DIM), k_g), cos, sin)
    v = v.reshape(bn, s_len, WG_KV_HEADS, HEAD_DIM)
    kc = rms_norm(kc.reshape(bn, l_len, WG_KV_HEADS, HEAD_DIM), k_g)
    vc = vc.reshape(bn, l_len, WG_KV_HEADS, HEAD_DIM)
    sink_l = sink.astype(F32).reshape(WG_KV_HEADS, WG_GROUP)

    pad = ((0, 0), (WG_BLOCK, WG_BLOCK), (0, 0), (0, 0))
    kp = jnp.pad(k, pad)
    vp = jnp.pad(v, pad)
    span = 3 * WG_BLOCK
    gidx = jnp.arange(nb)[:, None] * WG_BLOCK + jnp.arange(span)[None, :]
    kw = kp[:, gidx]
    vw = vp[:, gidx]
    qb = q.reshape(bn, nb, WG_BLOCK, WG_KV_HEADS, WG_GROUP, HEAD_DIM)
    s_ctx = jnp.einsum('bnihgd,bjhd->bnhgij', qb, kc).astype(F32) * scale
    s_win = jnp.einsum('bnihgd,bnjhd->bnhgij', qb, kw).astype(F32) * scale
    rel = jnp.arange(span)[None, :] - WG_BLOCK - jnp.arange(WG_BLOCK)[:, None]
    kpos = gidx - WG_BLOCK
    mask = (jnp.abs(rel) <= WINDOW)[None] & ((kpos >= 0) & (kpos < s_len))[:, None, :]
    s_win = jnp.where(mask[None, :, None, None], s_win, -jnp.inf)
    sink_col = jnp.broadcast_to(sink_l[None, None, :, :, None, None], s_ctx.shape[:-1] + (1,))
    p = jax.nn.softmax(jnp.concatenate([sink_col, s_ctx, s_win], axis=-1), axis=-1)
    p_ctx = p[..., 1:1 + l_len].astype(v.dtype)
    p_win = p[..., 1 + l_len:].astype(v.dtype)
    o = jnp.einsum('bnhgij,bjhd->bnihgd', p_ctx, vc) + jnp.einsum('bnhgij,bnjhd->bnihgd', p_win, vw)
    o = o.reshape(bn, s_len, BRANCH_W)

    oc = None
    if need_ctx:
        qcc = rms_norm(qc.reshape(bn, l_len, WG_KV_HEADS, WG_GROUP, HEAD_DIM), q_g)
        sc = jnp.einsum('bihgd,bjhd->bhgij', qcc, kc).astype(F32) * scale
        sink_c = jnp.broadcast_to(sink_l[None, :, :, None, None], sc.shape[:-1] + (1,))
        pc = jax.nn.softmax(jnp.concatenate([sink_c, sc], axis=-1), axis=-1)[..., 1:].astype(vc.dtype)
        oc = jnp.einsum('bhgij,bjhd->bihgd', pc, vc).reshape(bn, l_len, BRANCH_W)
    return o, oc


def depthwise_conv(x, w):
    return lax.conv_general_dilated(x, w[:, None, :], window_strides=(1,), padding='SAME',
                                    dimension_numbers=('NWC', 'WIO', 'NWC'),
                                    feature_group_count=x.shape[-1])


def gdn_chunk_scan(q, k, v, g, beta, state):
    bn, t_len, h = q.shape[:3]
    n = t_len // GD_CHUNK
    c = GD_CHUNK

    def chunks(a):
        a = a.reshape((bn, n, c, h) + a.shape[3:])
        return jnp.moveaxis(a, (1, 3), (0, 2))

    qc, kc, vc = chunks(q), chunks(k), chunks(v)
    gc = jnp.cumsum(chunks(g), axis=-1)
    bc = chunks(beta)
    incl = jnp.tril(jnp.ones((c, c), bool))
    strict = jnp.tril(jnp.ones((c, c), bool), -1)
    decay = jnp.exp(jnp.where(incl, gc[..., :, None] - gc[..., None, :], -jnp.inf))
    kb = kc * bc[..., None]
    a_mat = jnp.where(strict, jnp.einsum('nbhid,nbhjd->nbhij', kb, kc) * decay, 0.0)
    rhs = jnp.concatenate([vc * bc[..., None], kb * jnp.exp(gc)[..., None]], axis=-1)
    sol = lax.linalg.triangular_solve(a_mat + jnp.eye(c, dtype=F32), rhs, left_side=True, lower=True)
    u, w = sol[..., :GD_DV], sol[..., GD_DV:]
    qk = jnp.einsum('nbhid,nbhjd->nbhij', qc, kc) * decay
    qg = qc * jnp.exp(gc)[..., None]
    g_last = gc[..., -1]
    kd = kc * jnp.exp(g_last[..., None] - gc)[..., None]

    def step(s, xs):
        qg_i, qk_i, u_i, w_i, kd_i, gl_i = xs
        v_new = u_i - jnp.einsum('bhck,bhkv->bhcv', w_i, s)
        o = jnp.einsum('bhck,bhkv->bhcv', qg_i, s) + jnp.einsum('bhij,bhjv->bhiv', qk_i, v_new)
        s = s * jnp.exp(gl_i)[..., None, None] + jnp.einsum('bhck,bhcv->bhkv', kd_i, v_new)
        return s, o

    s_fin, o = lax.scan(step, state, (qg, qk, u, w, kd, g_last))
    o = jnp.moveaxis(o, (0, 2), (1, 3)).reshape(bn, t_len, h, GD_DV)
    return s_fin, o


def gated_deltanet(qkv, z, a, b, qkv_c, z_c, a_c, b_c, conv_w, a_log, dt_bias, norm_g, need_ctx):
    def prep(qkv_s, a_s, b_s):
        bn, t_len = qkv_s.shape[:2]
        y = jax.nn.silu(depthwise_conv(qkv_s, conv_w)).astype(F32)
        qq, kk, vv = jnp.split(y, [GD_HEADS * GD_DK, 2 * GD_HEADS * GD_DK], axis=-1)
        qq = l2_norm(qq.reshape(bn, t_len, GD_HEADS, GD_DK)) * (GD_DK ** -0.5)
        kk = l2_norm(kk.reshape(bn, t_len, GD_HEADS, GD_DK))
        vv = vv.reshape(bn, t_len, GD_HEADS, GD_DV)
        aa = a_s.astype(F32).reshape(bn, t_len, 2, GD_HEADS)
        gg = -jnp.exp(a_log.astype(F32)) * jax.nn.softplus(aa + dt_bias.astype(F32))
        bb = jax.nn.sigmoid(b_s.astype(F32).reshape(bn, t_len, 2, GD_HEADS))
        return qq, kk, vv, gg, bb

    ql, kl, vl, gl, bl = prep(qkv, a, b)
    qc, kc, vc, gc, bc = prep(qkv_c, a_c, b_c)
    zeros = jnp.zeros((qc.shape[0], GD_HEADS, GD_DK, GD_DV), F32)
    o_lat, o_ctx = 0.0, 0.0
    for d in range(2):
        f = (lambda t: jnp.flip(t, axis=1)) if d == 1 else (lambda t: t)
        s_c, oc_d = gdn_chunk_scan(f(qc), f(kc), f(vc), f(gc[:, :, d]), f(bc[:, :, d]), zeros)
        _, ol_d = gdn_chunk_scan(f(ql), f(kl), f(vl), f(gl[:, :, d]), f(bl[:, :, d]), s_c)
        o_lat = o_lat + f(ol_d)
        o_ctx = o_ctx + f(oc_d)

    def gated_out(o, zz):
        bn, t_len = o.shape[:2]
        zf = zz.reshape(bn, t_len, GD_HEADS, GD_DV).astype(F32)
        return (rms_norm(o, norm_g) * jax.nn.silu(zf)).reshape(bn, t_len, BRANCH_W).astype(zz.dtype)

    out = gated_out(o_lat, z)
    out_c = gated_out(o_ctx, z_c) if need_ctx else None
    return out, out_c


def merge_branches(outs, gate_logits, b_gate, w_branch, w_out):
    bn, t_len = gate_logits.shape[:2]
    o = jnp.stack(outs, axis=2)
    y = jnp.einsum('btrw,rwd->btrd', o, w_branch)
    gate = jax.nn.sigmoid(gate_logits + b_gate).reshape(bn, t_len, N_BRANCH, D_MODEL)
    return jnp.sum(gate * y, axis=2) @ w_out


def token_mixer(h, hc, cos, sin, w_in, b_gate, qk_g, diff_lam, diff_subln_g, wg_sink,
                gd_conv_w, gd_a_log, gd_dt_bias, gd_norm_g, w_branch, w_out, lam_init, need_ctx):
    qA, kA, vA, qB, kB, vB, qkvC, zC, aC, bC, gl = split_cols(h @ w_in)
    qAc, kAc, vAc, qBc, kBc, vBc, qkvCc, zCc, aCc, bCc, gc = split_cols(hc @ w_in)
    oA, oAc = diff_attention(qA, kA, vA, qAc, kAc, vAc, qk_g[0], qk_g[1], diff_lam, diff_subln_g,
                             lam_init, cos, sin, need_ctx)
    oB, oBc = window_gqa(qB, kB, vB, qBc, kBc, vBc, qk_g[2], qk_g[3], wg_sink, cos, sin, need_ctx)
    oC, oCc = gated_deltanet(qkvC, zC, aC, bC, qkvCc, zCc, aCc, bCc, gd_conv_w, gd_a_log,
                             gd_dt_bias, gd_norm_g, need_ctx)
    m = merge_branches((oA, oB, oC), gl, b_gate, w_branch, w_out)
    mc = merge_branches((oAc, oBc, oCc), gc, b_gate, w_branch, w_out) if need_ctx else None
    return m, mc


def peer_ffn(h, wq, keys, u_tab, v_tab):
    shp = h.shape
    t = h.reshape(-1, D_MODEL)
    n = t.shape[0]
    q = (t @ wq).reshape(n, PEER_HEADS, 2, PEER_HALF)
    s = jnp.einsum('thcd,cnd->thcn', q, keys).astype(F32)
    sv, si = lax.top_k(s, PEER_TOPK)
    cand = (sv[:, :, 0, :, None] + sv[:, :, 1, None, :]).reshape(n, PEER_HEADS, PEER_TOPK * PEER_TOPK)
    cv, ci = lax.top_k(cand, PEER_TOPK)
    i1 = jnp.take_along_axis(si[:, :, 0], ci // PEER_TOPK, axis=-1)
    i2 = jnp.take_along_axis(si[:, :, 1], ci % PEER_TOPK, axis=-1)
    eid = (i1 * N_KEYS + i2).reshape(n, PEER_HEADS * PEER_TOPK)
    gw = jax.nn.softmax(cv, axis=-1).reshape(n, PEER_HEADS * PEER_TOPK).astype(h.dtype)
    nb = n // PEER_BLOCK

    def block(args):
        tb, eb, gb = args
        act = jax.nn.gelu(jnp.einsum('pd,ped->pe', tb, u_tab[eb]), approximate=False)
        return jnp.einsum('pe,ped->pd', act * gb, v_tab[eb])

    out = lax.map(block, (t.reshape(nb, PEER_BLOCK, D_MODEL),
                          eid.reshape(nb, PEER_BLOCK, -1),
                          gw.reshape(nb, PEER_BLOCK, -1)))
    return out.reshape(shp)


def setup_inputs(seed: int = 0) -> dict:
    key = jax.random.key(seed)
    ks = jax.random.split(key, 24)
    D = D_MODEL

    def nrm(k, shape, s):
        return jax.random.normal(k, shape, F32) * s

    dt = jnp.exp(jax.random.uniform(ks[15], (DEPTH, 2, GD_HEADS), F32)
                 * (math.log(0.1) - math.log(0.001)) + math.log(0.001))
    return {
        'x': nrm(ks[0], (BATCH, SEQ, D), 1.0),
        'c': nrm(ks[1], (BATCH, D), 1.0),
        'ctx': nrm(ks[2], (BATCH, CTX_LEN, D), 1.0),
        'c_ctx': nrm(ks[3], (D,), 1.0),
        'norm1_g': 1.0 + nrm(ks[4], (DEPTH, D), 0.02),
        'norm2_g': 1.0 + nrm(ks[5], (DEPTH, D), 0.02),
        'w_ada': nrm(ks[6], (DEPTH, D, 6 * D), 0.5 * D ** -0.5),
        'b_ada': nrm(ks[7], (DEPTH, 6 * D), 0.02),
        'w_in': nrm(ks[8], (DEPTH, D, IN_COLS), D ** -0.5),
        'b_gate': nrm(ks[9], (DEPTH, N_BRANCH * D), 0.02),
        'qk_norm_g': 1.0 + nrm(ks[10], (DEPTH, 4, HEAD_DIM), 0.02),
        'diff_lam': nrm(ks[11], (DEPTH, 2, 2, HEAD_DIM), 0.1),
        'diff_subln_g': 1.0 + nrm(ks[12], (DEPTH, DA_VDIM), 0.02),
        'wg_sink': nrm(ks[13], (DEPTH, WG_HEADS), 1.0),
        'gd_conv_w': nrm(ks[14], (DEPTH, GD_CONV, GD_QKV_W), GD_CONV ** -0.5),
        'gd_a_log': jnp.log(jax.random.uniform(ks[16], (DEPTH, 2, GD_HEADS), F32, 1.0, 16.0)),
        'gd_dt_bias': dt + jnp.log(-jnp.expm1(-dt)),
        'gd_norm_g': 1.0 + nrm(ks[17], (DEPTH, GD_DV), 0.02),
        'w_branch': nrm(ks[18], (DEPTH, N_BRANCH, BRANCH_W, D), BRANCH_W ** -0.5),
        'w_out': nrm(ks[19], (DEPTH, D, D), D ** -0.5),
        'peer_wq': nrm(ks[20], (DEPTH, D, PEER_HEADS * PEER_DQ), D ** -0.5),
        'peer_keys': nrm(ks[21], (DEPTH, 2, N_KEYS, PEER_HALF), PEER_HALF ** -0.5),
        'peer_u': nrm(ks[22], (DEPTH, N_EXPERTS, D), D ** -0.5),
        'peer_v': nrm(ks[23], (DEPTH, N_EXPERTS, D), PEER_HEADS ** -0.5),
    }


def reference(x, c, ctx, c_ctx, norm1_g, norm2_g, w_ada, b_ada, w_in, b_gate, qk_norm_g,
              diff_lam, diff_subln_g, wg_sink, gd_conv_w, gd_a_log, gd_dt_bias, gd_norm_g,
              w_branch, w_out, peer_wq, peer_keys, peer_u, peer_v):
    s_len = x.shape[1]
    rows = s_len // GRID_W
    cos, sin = rope_2d_tables(rows)
    xc = ctx
    for l in range(DEPTH):
        need_ctx = l < DEPTH - 1
        lam_init = 0.8 - 0.6 * math.exp(-0.3 * l)
        sh1, sc1, gt1, sh2, sc2, gt2 = ada_mod(c, w_ada[l], b_ada[l])
        sh1c, sc1c, gt1c, sh2c, sc2c, gt2c = ada_mod(c_ctx[None], w_ada[l], b_ada[l])
        h = modulate(x, norm1_g[l], sh1, sc1)
        hc = modulate(xc, norm1_g[l], sh1c, sc1c)
        m, mc = token_mixer(h, hc, cos, sin, w_in[l], b_gate[l], qk_norm_g[l], diff_lam[l],
                            diff_subln_g[l], wg_sink[l], gd_conv_w[l], gd_a_log[l], gd_dt_bias[l],
                            gd_norm_g[l], w_branch[l], w_out[l], lam_init, need_ctx)
        x = x + gt1 * m
        x = x + gt2 * peer_ffn(modulate(x, norm2_g[l], sh2, sc2), peer_wq[l], peer_keys[l],
                               peer_u[l], peer_v[l])
        if need_ctx:
            xc = xc + gt1c * mc
            xc = xc + gt2c * peer_ffn(modulate(xc, norm2_g[l], sh2c, sc2c), peer_wq[l],
                                      peer_keys[l], peer_u[l], peer_v[l])
    return x
```

```python
import numpy as np
from contextlib import ExitStack
import concourse.bass as bass
import concourse.mybir as mybir

F32 = mybir.dt.float32
BF16 = mybir.dt.bfloat16
I32 = mybir.dt.int32
U32 = mybir.dt.uint32
AF = mybir.ActivationFunctionType
ALU = mybir.AluOpType
AX = mybir.AxisListType


class Buf:
    __slots__ = ("name", "w", "r", "excl")

    def __init__(self, name, excl=False):
        self.name = name
        self.w = None
        self.r = {}
        self.excl = excl


class KB:
    ENG = ("pe", "dve", "act", "pool", "sp")

    def __init__(self, nc, es):
        self.nc = nc
        self.es = es
        self.eng = {"pe": nc.tensor, "dve": nc.vector, "act": nc.scalar, "pool": nc.gpsimd, "sp": nc.sync}
        self.ops = {e: [] for e in self.ENG}
        self.cnt = {e: 0 for e in self.ENG}
        self.seen = {e: {} for e in self.ENG}
        self.csem = {e: es.enter_context(nc.semaphore("c_" + e)) for e in self.ENG}
        self.NS = 8
        self.DQ = ("sp", "pool")
        self.dsem = {e: [es.enter_context(nc.semaphore(f"d_{e}{j}")) for j in range(self.NS)] for e in self.DQ}
        self.dcnt = {e: [0] * self.NS for e in self.DQ}
        self.dtot = {e: 0 for e in self.DQ}
        self.nbuf = 0

    def sb(self, name, shape, dt=F32):
        self.nbuf += 1
        name = f"s{self.nbuf}_{name}"
        t = self.es.enter_context(self.nc.sbuf_tensor(name, list(shape), dt))
        return t, Buf(name)

    def ps(self, name, shape, dt=F32):
        self.nbuf += 1
        name = f"p{self.nbuf}_{name}"
        t = self.es.enter_context(self.nc.psum_tensor(name, list(shape), dt))
        return t, Buf(name, excl=True)

    def dram(self, name, shape, dt=F32, kind="Internal"):
        t = self.nc.dram_tensor(name, list(shape), dt, kind=kind)
        return t.ap(), Buf(name)

    def _waits(self, E, reads, writes):
        toks = []
        for b in reads:
            if b.w is not None:
                toks.append(b.w)
        for b in writes:
            if b.w is not None:
                toks.append(b.w)
            toks.extend(b.r.values())
        waits = {}
        for kind, E2, idx in toks:
            if kind == "c":
                if E2 == E and E == "pe":
                    continue
                sem, val, key = self.csem[E2], idx, "c_" + E2
            else:
                j, m = idx
                sem, val, key = self.dsem[E2][j], 16 * m, f"d_{E2}{j}"
            if self.seen[E].get(key, 0) >= val:
                continue
            if key not in waits or waits[key][1] < val:
                waits[key] = (sem, val)
        for key, (sem, val) in waits.items():
            self.seen[E][key] = val
        return list(waits.values())

    def op(self, E, fn, reads=(), writes=()):
        ex = [b for b in reads if b.excl]
        if ex:
            reads = [b for b in reads if not b.excl]
            writes = list(writes) + ex
        waits = self._waits(E, reads, writes)
        self.cnt[E] += 1
        idx = self.cnt[E]
        self.ops[E].append((waits, fn, self.csem[E], 1))
        tok = ("c", E, idx)
        for b in reads:
            b.r[("c", E)] = tok
        for b in writes:
            b.w = tok
            b.r = {}

    def dma(self, Q, fn, reads=(), writes=()):
        waits = self._waits(Q, reads, writes)
        j = self.dtot[Q] % self.NS
        self.dtot[Q] += 1
        key = f"d_{Q}{j}"
        if self.dcnt[Q][j] > 0 and self.seen[Q].get(key, 0) < 16 * self.dcnt[Q][j]:
            waits.append((self.dsem[Q][j], 16 * self.dcnt[Q][j]))
            self.seen[Q][key] = 16 * self.dcnt[Q][j]
        self.dcnt[Q][j] += 1
        self.ops[Q].append((waits, fn, self.dsem[Q][j], 16))
        tok = ("d", Q, (j, self.dcnt[Q][j]))
        for b in reads:
            b.r[("d", Q, j)] = tok
        for b in writes:
            b.w = tok
            b.r = {}

    def barrier(self):
        for E in self.ENG:
            waits = []
            for E2 in self.ENG:
                if E2 != E and self.cnt[E2] > self.seen[E].get("c_" + E2, 0):
                    waits.append((self.csem[E2], self.cnt[E2]))
                    self.seen[E]["c_" + E2] = self.cnt[E2]
                if E2 in self.DQ:
                    for j in range(self.NS):
                        key = f"d_{E2}{j}"
                        if self.dcnt[E2][j] * 16 > self.seen[E].get(key, 0):
                            waits.append((self.dsem[E2][j], 16 * self.dcnt[E2][j]))
                            self.seen[E][key] = 16 * self.dcnt[E2][j]
            if E != "pe" and self.cnt[E] > self.seen[E].get("c_" + E, 0):
                waits.append((self.csem[E], self.cnt[E]))
                self.seen[E]["c_" + E] = self.cnt[E]
            if waits:
                self.ops[E].append((waits, None, None, 0))

    def emit(self):
        self.barrier()
        nc = self.nc
        with nc.Block() as block:
            def mk(E):
                def body(eng):
                    for waits, fn, sem, inc in self.ops[E]:
                        for s, v in waits:
                            eng.wait_ge(s, v)
                        if fn is not None:
                            fn(eng).then_inc(sem, inc)
                return body
            block.tensor(mk("pe"))
            block.vector(mk("dve"))
            block.scalar(mk("act"))
            block.gpsimd(mk("pool"))
            block.sync(mk("sp"))

    def ninstr(self):
        return {e: len(self.ops[e]) for e in self.ENG}


import math
import numpy as np
from contextlib import ExitStack

D = 1024
KD = 8
IN_COLS = 7440
C_QA, C_KA, C_VA, C_QB, C_KB, C_VB, C_QKVC, C_ZC, C_AC, C_BC, C_GL = 0, 512, 1024, 1536, 2048, 2176, 2304, 3840, 4352, 4360, 4368
EPS = 1e-6


class StopPhase(Exception):
    pass


class M:
    def __init__(self, cfg):
        self.cfg = cfg
        self.NB = cfg["NB"]; self.S = cfg["S"]; self.L = cfg["L"]; self.DEPTH = cfg["DEPTH"]
        self.T = self.S + self.L
        self.NT = self.T // 128
        self.NLT = self.L // 128
        self.stop_after = cfg.get("stop_after", "all")

    def decl_inputs(self, nc):
        NB, S, L, DEPTH = self.NB, self.S, self.L, self.DEPTH
        shapes = dict(
            x=[NB, S, D], c=[NB, D], ctx=[NB, L, D], c_ctx=[D], norm1_g=[DEPTH, D], norm2_g=[DEPTH, D],
            w_ada=[DEPTH, D, 6 * D], b_ada=[DEPTH, 6 * D], w_in=[DEPTH, D, IN_COLS], b_gate=[DEPTH, 3 * D],
            qk_norm_g=[DEPTH, 4, 64], diff_lam=[DEPTH, 2, 2, 64], diff_subln_g=[DEPTH, 128], wg_sink=[DEPTH, 8],
            gd_conv_w=[DEPTH, 5, 1536], gd_a_log=[DEPTH, 2, 4], gd_dt_bias=[DEPTH, 2, 4], gd_norm_g=[DEPTH, 128],
            w_branch=[DEPTH, 3, 512, D], w_out=[DEPTH, D, D], peer_wq=[DEPTH, D, 2048], peer_keys=[DEPTH, 2, 128, 128],
            peer_uv=[DEPTH * 16384, 2 * D],
            ident=[128, 128], rope_cos=[S, 64], rope_sin=[S, 64], mask_lo=[128, 128], mask_hi=[128, 128], gmasks=[8, 128, 128],
        )
        self.inp = {n: nc.dram_tensor(n, s, F32, kind="ExternalInput").ap() for n, s in shapes.items()}
        self.out = nc.dram_tensor("out", [NB, S, D], F32, kind="ExternalOutput").ap()

    def build(self):
        nc = bass.Bass("TRN2", target_bir_lowering=False)
        self.nc = nc
        self.decl_inputs(nc)
        with ExitStack() as es:
            k = KB(nc, es)
            self.k = k
            k.es0 = es
            self.alloc()
            self.prologue()
            for l in range(self.DEPTH):
                self.layer(l)
            self.epilogue()
            k.emit()
            print("instr counts", k.ninstr())
        return nc

    def alloc(self):
        k = self.k; NB, T = self.NB, self.T
        dk = "ExternalOutput" if self.cfg.get("debug") else "Internal"
        self.xs, self.xs_b = k.dram("xs", [NB, T, D], kind=dk)
        self.mod_d, self.mod_b = k.dram("mod_d", [NB + 1, 6 * D], kind=dk)
        self.proj_d, self.proj_b = k.dram("proj_d", [1, T, IN_COLS], kind=dk)
        self.qkvT_d, self.qkvT_b = k.dram("qkvT_d", [1, 1536, T], kind=dk)
        self.oT_d, self.oT_b = k.dram("oT_d", [1, 1536, T], BF16, kind=dk)
        self.uvb, self.uvb_b = k.dram("uvb", [16384, 2 * D], BF16)
        self.ident, self.ident_b = k.sb("ident", [128, 128])
        self.identb, self.identb_b = k.sb("identb", [128, 128], BF16)
        self.ev = 0

    def psum_std(self):
        k = self.k
        self.psA = [k.ps(f"psA{i}", [128, 512]) for i in range(6)]
        self.psT = [k.ps(f"psT{i}", [128, 8, 128], BF16) for i in range(2)]

    def evac(self, out_ap, in_ap, reads, writes, func=None):
        k = self.k
        self.ev ^= 1
        if self.ev:
            k.op("act", lambda e: e.activation(out=out_ap, in_=in_ap, func=AF.Copy), reads, writes)
        else:
            k.op("dve", lambda e: e.tensor_copy(out=out_ap, in_=in_ap), reads, writes)

    def prologue(self):
        k = self.k; inp = self.inp; NB, L, S = self.NB, self.L, self.S
        k.dma("sp", lambda e: e.dma_start(out=self.ident[:], in_=inp["ident"][:, :]), [], [self.ident_b])
        k.op("dve", lambda e: e.tensor_copy(out=self.identb[:], in_=self.ident[:]), [self.ident_b], [self.identb_b])
        for b in range(NB):
            k.dma("sp", lambda e, b=b: e.dma_start(out=self.xs[b, 0:L, :], in_=inp["ctx"][b, :, :]), [], [self.xs_b])
            k.dma("sp", lambda e, b=b: e.dma_start(out=self.xs[b, L:L + S, :], in_=inp["x"][b, :, :]), [], [self.xs_b])

    def epilogue(self):
        k = self.k; NB, L, S = self.NB, self.L, self.S
        for b in range(NB):
            k.dma("sp", lambda e, b=b: e.dma_start(out=self.out[b, :, :], in_=self.xs[b, L:L + S, :]), [self.xs_b], [])

    def phase_T(self, l):
        k = self.k; inp = self.inp
        k.barrier()
        with ExitStack() as es:
            k.es = es
            RB = 2
            tf = [k.sb(f"tf{i}", [128, RB, 2 * D]) for i in range(3)]
            tb = [k.sb(f"tb{i}", [128, RB, 2 * D], BF16) for i in range(3)]
            engs = ("dve", "act", "pool")
            for i, r0 in enumerate(range(0, 16384, 128 * RB)):
                f_, fb_ = tf[i % 3]; b_, bb_ = tb[i % 3]
                src = inp["peer_uv"][l * 16384 + r0:l * 16384 + r0 + 128 * RB, :].rearrange("(p j) c -> p j c", j=RB)
                dst = self.uvb[r0:r0 + 128 * RB, :].rearrange("(p j) c -> p j c", j=RB)
                k.dma("sp", lambda e, f_=f_, src=src: e.dma_start(out=f_[:], in_=src), [], [fb_])
                E_ = engs[i % 3]
                if E_ == "act":
                    k.op("act", lambda e, f_=f_, b_=b_: e.activation(out=b_[:], in_=f_[:], func=AF.Copy), [fb_], [bb_])
                else:
                    k.op(E_, lambda e, f_=f_, b_=b_: e.tensor_copy(out=b_[:], in_=f_[:]), [fb_], [bb_])
                k.dma("sp", lambda e, b_=b_, dst=dst: e.dma_start(out=dst, in_=b_[:]), [bb_], [self.uvb_b])
        k.es = k.es0
        k.barrier()

    def layer(self, l):
        self.phase_ada(l)
        if self.stop_after not in ("ada", "proj", "A", "B", "C", "M"):
            self.phase_T(l)
        if self.stop_after == "ada":
            return
        for b in range(self.NB):
            self.phase_proj(l, b)
            if self.stop_after == "proj":
                continue
            self.phase_A(l, b)
            if self.stop_after == "A":
                continue
            self.phase_B(l, b)
            if self.stop_after == "B":
                continue
            self.phase_C(l, b)
            if self.stop_after == "C":
                continue
            self.phase_M(l, b)
            if self.stop_after == "M":
                continue
            self.phase_P(l, b)

    def phase_ada(self, l):
        k = self.k; inp = self.inp; NB = self.NB; R = NB + 1
        k.barrier()
        with ExitStack() as es:
            k.es = es
            self.psum_std()
            cT, cTb = k.sb("cT", [128, KD, R])
            wt = [k.sb(f"adaw{i}", [128, KD, 512]) for i in range(2)]
            bt = [k.sb(f"adab{i}", [R, 512]) for i in range(2)]
            ot = [k.sb(f"adao{i}", [R, 512]) for i in range(2)]
            for n_ in range(NB):
                k.dma("sp", lambda e, n_=n_: e.dma_start(out=cT[:, :, n_:n_ + 1], in_=inp["c"][n_, :].rearrange("(k p o) -> p k o", p=128, o=1), allow_slow_non_contiguous=True), [], [cTb])
            k.dma("sp", lambda e: e.dma_start(out=cT[:, :, NB:R], in_=inp["c_ctx"].rearrange("(k p o) -> p k o", p=128, o=1), allow_slow_non_contiguous=True), [], [cTb])
            k.op("act", lambda e: e.activation(out=cT[:], in_=cT[:], func=AF.Silu), [cTb], [cTb])
            for cb in range(12):
                w, wb = wt[cb % 2]; bb, bbb = bt[cb % 2]; oo, oob = ot[cb % 2]
                ps, psb = self.psA[cb % 2]
                c0 = cb * 512
                k.dma("sp", lambda e, w=w, c0=c0: e.dma_start(out=w[:], in_=inp["w_ada"][l, :, c0:c0 + 512].rearrange("(k p) c -> p k c", p=128)), [], [wb])
                k.dma("pool", lambda e, bb=bb, c0=c0: e.dma_start(out=bb[:], in_=inp["b_ada"][l, c0:c0 + 512].partition_broadcast(R)), [], [bbb])
                for kk in range(KD):
                    k.op("pe", lambda e, kk=kk, w=w, ps=ps: e.matmul(ps[0:R, :], lhsT=cT[:, kk, :], rhs=w[:, kk, :], start=(kk == 0), stop=(kk == KD - 1)), [cTb, wb], [psb])
                k.op("dve", lambda e, oo=oo, ps=ps, bb=bb: e.tensor_tensor(out=oo[:], in0=ps[0:R, :], in1=bb[:], op=ALU.add), [psb, bbb], [oob])
                k.dma("sp", lambda e, oo=oo, c0=c0: e.dma_start(out=self.mod_d[:, c0:c0 + 512], in_=oo[:]), [oob], [self.mod_b])
        k.es = k.es0
        k.barrier()

    def mod_tile(self, es, name, l, r, j, plus_one=False, gname=None):
        k = self.k; inp = self.inp
        t, tb = k.sb(name, [128, D])
        k.dma("sp", lambda e: e.dma_start(out=t[:], in_=self.mod_d[r, j * D:(j + 1) * D].partition_broadcast(128)), [self.mod_b], [tb])
        if plus_one:
            g, gb = k.sb(name + "_g", [128, D])
            k.dma("pool", lambda e: e.dma_start(out=g[:], in_=inp[gname][l, :].partition_broadcast(128)), [], [gb])
            k.op("dve", lambda e: e.scalar_tensor_tensor(out=t[:], in0=t[:], scalar=1.0, in1=g[:], op0=ALU.add, op1=ALU.mult), [tb, gb], [tb])
        return t, tb

    def rstd(self, ss, ssb, n, reads=()):
        k = self.k
        k.op("act", lambda e: e.activation(out=ss, in_=ss, func=AF.Ln, scale=1.0 / n, bias=EPS), [ssb], [ssb])
        k.op("act", lambda e: e.activation(out=ss, in_=ss, func=AF.Exp, scale=-0.5), [ssb], [ssb])

    def phase_proj(self, l, b):
        k = self.k; inp = self.inp; NB, T, NT, NLT = self.NB, self.T, self.NT, self.NLT
        k.barrier()
        with ExitStack() as es:
            k.es = es
            self.psum_std()
            hT, hTb = k.sb("hT", [128, KD, T], BF16)
            A1 = {}; B1 = {}
            for r, nm in ((b, "lat"), (NB, "ctx")):
                A1[nm] = self.mod_tile(es, "A1" + nm, l, r, 1, True, "norm1_g")
                B1[nm] = self.mod_tile(es, "B1" + nm, l, r, 0)
            xt = [k.sb(f"xt{i}", [128, D]) for i in range(2)]
            junk, junkb = k.sb("junk", [128, D])
            hh = [k.sb(f"hh{i}", [128, D], BF16) for i in range(2)]
            sst = [k.sb(f"ss{i}", [128, 1]) for i in range(2)]
            for tt in range(NT):
                nm = "ctx" if tt < NLT else "lat"
                x_, xb_ = xt[tt % 2]; h_, hb_ = hh[tt % 2]; s_, sb_ = sst[tt % 2]
                pt, ptb = self.psT[tt % 2]
                t0 = tt * 128
                k.dma("sp", lambda e, x_=x_, t0=t0: e.dma_start(out=x_[:], in_=self.xs[b, t0:t0 + 128, :]), [self.xs_b], [xb_])
                k.op("act", lambda e, x_=x_, s_=s_: e.activation(out=junk[:], in_=x_[:], func=AF.Square, accum_out=s_[:]), [xb_], [junkb, sb_])
                self.rstd(s_[:], sb_, D)
                a_, ab_ = A1[nm]; b_, bb_ = B1[nm]
                k.op("dve", lambda e, x_=x_, s_=s_, a_=a_: e.scalar_tensor_tensor(out=x_[:], in0=x_[:], scalar=s_[:, 0:1], in1=a_[:], op0=ALU.mult, op1=ALU.mult), [xb_, sb_, ab_], [xb_])
                k.op("pool", lambda e, x_=x_, h_=h_, b_=b_: e.tensor_tensor(out=h_[:], in0=x_[:], in1=b_[:], op=ALU.add), [xb_, bb_], [hb_])
                for kk in range(KD):
                    k.op("pe", lambda e, kk=kk, h_=h_, pt=pt: e.transpose(out=pt[:, kk, :], in_=h_[:, kk * 128:(kk + 1) * 128], identity=self.identb[:]), [hb_, self.identb_b], [ptb])
                self.evac(hT[:, :, t0:t0 + 128], pt[:], [ptb], [hTb])
            wf = [k.sb(f"wf{i}", [128, KD, 512]) for i in range(2)]
            wb16 = [k.sb(f"wb{i}", [128, KD, 512], BF16) for i in range(2)]
            st = [k.sb(f"st{i}", [128, 512]) for i in range(3)]
            blocks = []
            for (a, z) in ((0, C_QKVC), (C_ZC, IN_COLS)):
                c0 = a
                while c0 < z:
                    cw = min(512, z - c0); blocks.append((c0, cw)); c0 += cw
            n = 0
            for bi, (c0, cw) in enumerate(blocks):
                w, wb_ = wf[bi % 2]; w16, w16b = wb16[bi % 2]
                k.dma("sp", lambda e, w=w, c0=c0, cw=cw: e.dma_start(out=w[:, :, 0:cw], in_=inp["w_in"][l, :, c0:c0 + cw].rearrange("(k p) c -> p k c", p=128)), [], [wb_])
                k.op("pool", lambda e, w=w, w16=w16, cw=cw: e.tensor_copy(out=w16[:, :, 0:cw], in_=w[:, :, 0:cw]), [wb_], [w16b])
                for tt in range(NT):
                    ps, psb = self.psA[n % 4]; s_, sb_ = st[n % 3]; n += 1
                    t0 = tt * 128
                    for kk in range(KD):
                        k.op("pe", lambda e, kk=kk, ps=ps, w16=w16, t0=t0, cw=cw: e.matmul(ps[:, 0:cw], lhsT=hT[:, kk, t0:t0 + 128], rhs=w16[:, kk, 0:cw], start=(kk == 0), stop=(kk == KD - 1)), [hTb, w16b], [psb])
                    self.evac(s_[:, 0:cw], ps[:, 0:cw], [psb], [sb_])
                    k.dma("sp", lambda e, s_=s_, t0=t0, c0=c0, cw=cw: e.dma_start(out=self.proj_d[0, t0:t0 + 128, c0:c0 + cw], in_=s_[:, 0:cw]), [sb_], [self.proj_b])
            TB = 512
            for chb in range(12):
                w, wb_ = wf[chb % 2]; w16, w16b = wb16[chb % 2]
                c0 = C_QKVC + chb * 128
                k.dma("sp", lambda e, w=w, c0=c0: e.dma_start(out=w[:, :, 0:128], in_=inp["w_in"][l, :, c0:c0 + 128].rearrange("(k p) c -> p k c", p=128)), [], [wb_])
                k.op("pool", lambda e, w=w, w16=w16: e.tensor_copy(out=w16[:, :, 0:128], in_=w[:, :, 0:128]), [wb_], [w16b])
                for t0 in range(0, T, TB):
                    tn = min(TB, T - t0)
                    ps, psb = self.psA[n % 4]; s_, sb_ = st[n % 3]; n += 1
                    for kk in range(KD):
                        k.op("pe", lambda e, kk=kk, ps=ps, w16=w16, t0=t0, tn=tn: e.matmul(ps[:, 0:tn], lhsT=w16[:, kk, 0:128], rhs=hT[:, kk, t0:t0 + tn], start=(kk == 0), stop=(kk == KD - 1)), [hTb, w16b], [psb])
                    self.evac(s_[:, 0:tn], ps[:, 0:tn], [psb], [sb_])
                    k.dma("sp", lambda e, s_=s_, t0=t0, chb=chb, tn=tn: e.dma_start(out=self.qkvT_d[0, chb * 128:(chb + 1) * 128, t0:t0 + tn], in_=s_[:, 0:tn]), [sb_], [self.qkvT_b])
        k.es = k.es0
        k.barrier()

    def phase_A(self, l, b):
        k = self.k; inp = self.inp; NB, T, NT, NLT, L, S = self.NB, self.T, self.NT, self.NLT, self.L, self.S
        lam_init = 0.8 - 0.6 * math.exp(-0.3 * l)
        k.barrier()
        with ExitStack() as es:
            k.es = es
            self.psum_std()
            qT, qTb = k.sb("qT", [128, 4, T], BF16)
            kT, kTb = k.sb("kT", [128, 4, T], BF16)
            vA, vAb = k.sb("vA", [128, NT, 512], BF16)
            onesb, onesbb = k.sb("onesb", [128, 128], BF16)
            onesf, onesfb = k.sb("onesf", [128, 128])
            k.op("pool", lambda e: e.memset(onesb[:], 1.0), [], [onesbb])
            k.op("pool", lambda e: e.memset(onesf[:], 1.0), [], [onesfb])
            lv, lvb = k.sb("lv", [128, 256]); lp, lpb = k.sb("lp", [128, 128]); le, leb = k.sb("le", [128, 2]); nlam, nlamb = k.sb("nlam", [128, 1])
            k.dma("sp", lambda e: e.dma_start(out=lv[:], in_=inp["diff_lam"][l].rearrange("a b c -> (a b c)").partition_broadcast(128)), [], [lvb])
            lv4 = lv[:].rearrange("p (m j c) -> p m j c", m=2, j=2)
            k.op("dve", lambda e: e.tensor_tensor(out=lp[:].rearrange("p (m c) -> p m c", m=2), in0=lv4[:, :, 0, :], in1=lv4[:, :, 1, :], op=ALU.mult), [lvb], [lpb])
            k.op("dve", lambda e: e.tensor_reduce(out=le[:], in_=lp[:].rearrange("p (m c) -> p m c", m=2), axis=AX.X, op=ALU.add), [lpb], [leb])
            k.op("act", lambda e: e.activation(out=le[:], in_=le[:], func=AF.Exp), [leb], [leb])
            k.op("dve", lambda e: e.scalar_tensor_tensor(out=nlam[:], in0=le[:, 1:2], scalar=-lam_init, in1=le[:, 0:1], op0=ALU.add, op1=ALU.subtract), [leb], [nlamb])
            gqk, gqkb = k.sb("gqk", [128, 16, 64])
            for g in range(16):
                k.dma("pool", lambda e, g=g: e.dma_start(out=gqk[:, g, :], in_=inp["qk_norm_g"][l, 0 if g < 8 else 1, :].partition_broadcast(128)), [], [gqkb])
            sg, sgb = k.sb("sg", [128, 1])
            k.dma("sp", lambda e: e.dma_start(out=sg[:], in_=inp["diff_subln_g"][l, :].rearrange("(p o) -> p o", o=1)), [], [sgb])
            self.qk_prep(l, b, C_QA, 16, gqk, gqkb, [(qT, qTb, 0, 4), (kT, kTb, 8, 4)])
            vf = [k.sb(f"vf{i}", [128, 512]) for i in range(2)]
            for tt in range(NT):
                v_, vb_ = vf[tt % 2]
                k.dma("sp", lambda e, v_=v_, tt=tt: e.dma_start(out=v_[:], in_=self.proj_d[0, tt * 128:(tt + 1) * 128, C_VA:C_VA + 512]), [self.proj_b], [vb_])
                k.op("pool", lambda e, v_=v_, tt=tt: e.tensor_copy(out=vA[:, tt, :], in_=v_[:]), [vb_], [vAb])
            pT = [k.sb(f"pT{i}", [128, 512], BF16) for i in range(3)]
            rs = [k.sb(f"rs{i}", [128, 512]) for i in range(2)]
            o1, o1b = k.sb("o1", [128, 512]); o2, o2b = k.sb("o2", [128, 512]); sq, sqb = k.sb("sq", [128, 512])
            ob = [k.sb(f"ob{i}", [128, 512], BF16) for i in range(2)]
            qblocks = [(0, L, 0, NLT)] + [(q0, min(512, T - q0), 0, NT) for q0 in range(L, T, 512)]
            n = 0; nob = 0
            for h in range(4):
                for (q0, qn, kt0, kt1) in qblocks:
                    psO = [self.psA[2], self.psA[3]]; psS = [self.psA[4], self.psA[5]]
                    for kt in range(kt0, kt1):
                        for m in range(2):
                            ps, psb = self.psA[n % 2]; p_, pb_ = pT[n % 3]; n += 1
                            k.op("pe", lambda e, ps=ps, m=m, kt=kt, h=h, q0=q0, qn=qn: e.matmul(ps[:, 0:qn], lhsT=kT[64 * m:64 * m + 64, h, kt * 128:(kt + 1) * 128], rhs=qT[64 * m:64 * m + 64, h, q0:q0 + qn], start=True, stop=True), [kTb, qTb], [psb])
                            k.op("act", lambda e, ps=ps, p_=p_, qn=qn: e.activation(out=p_[:, 0:qn], in_=ps[:, 0:qn], func=AF.Exp, scale=0.125), [psb], [pb_])
                            k.op("pe", lambda e, m=m, kt=kt, h=h, p_=p_, qn=qn, kt0=kt0, kt1=kt1: e.matmul(psO[m][0][:, 0:qn], lhsT=vA[:, kt, h * 128:(h + 1) * 128], rhs=p_[:, 0:qn], start=(kt == kt0), stop=(kt == kt1 - 1)), [vAb, pb_], [psO[m][1]])
                            k.op("pe", lambda e, m=m, kt=kt, p_=p_, qn=qn, kt0=kt0, kt1=kt1: e.matmul(psS[m][0][:, 0:qn], lhsT=onesb[:], rhs=p_[:, 0:qn], start=(kt == kt0), stop=(kt == kt1 - 1)), [onesbb, pb_], [psS[m][1]])
                    for m in range(2):
                        k.op("dve", lambda e, m=m, qn=qn: e.reciprocal(out=rs[m][0][:, 0:qn], in_=psS[m][0][:, 0:qn]), [psS[m][1]], [rs[m][1]])
                    k.op("dve", lambda e, qn=qn: e.tensor_tensor(out=o1[:, 0:qn], in0=psO[0][0][:, 0:qn], in1=rs[0][0][:, 0:qn], op=ALU.mult), [psO[0][1], rs[0][1]], [o1b])
                    k.op("dve", lambda e, qn=qn: e.tensor_tensor(out=o2[:, 0:qn], in0=psO[1][0][:, 0:qn], in1=rs[1][0][:, 0:qn], op=ALU.mult), [psO[1][1], rs[1][1]], [o2b])
                    k.op("dve", lambda e, qn=qn: e.scalar_tensor_tensor(out=o1[:, 0:qn], in0=o2[:, 0:qn], scalar=nlam[:, 0:1], in1=o1[:, 0:qn], op0=ALU.mult, op1=ALU.add), [o2b, nlamb, o1b], [o1b])
                    k.op("act", lambda e, qn=qn: e.activation(out=sq[:, 0:qn], in_=o1[:, 0:qn], func=AF.Square), [o1b], [sqb])
                    ps, psb = self.psA[n % 2]; n += 1
                    k.op("pe", lambda e, ps=ps, qn=qn: e.matmul(ps[:, 0:qn], lhsT=onesf[:], rhs=sq[:, 0:qn], start=True, stop=True), [onesfb, sqb], [psb])
                    k.op("act", lambda e, ps=ps, qn=qn: e.activation(out=sq[:, 0:qn], in_=ps[:, 0:qn], func=AF.Ln, scale=1.0 / 128, bias=EPS), [psb], [sqb])
                    k.op("act", lambda e, qn=qn: e.activation(out=sq[:, 0:qn], in_=sq[:, 0:qn], func=AF.Exp, scale=-0.5), [sqb], [sqb])
                    k.op("dve", lambda e, qn=qn: e.tensor_tensor(out=o1[:, 0:qn], in0=o1[:, 0:qn], in1=sq[:, 0:qn], op=ALU.mult), [o1b, sqb], [o1b])
                    o_, ob_ = ob[nob % 2]; nob += 1
                    k.op("dve", lambda e, o_=o_, qn=qn: e.tensor_scalar(out=o_[:, 0:qn], in0=o1[:, 0:qn], scalar1=sg[:, 0:1], scalar2=(1.0 - lam_init), op0=ALU.mult, op1=ALU.mult), [o1b, sgb], [ob_])
                    k.dma("sp", lambda e, o_=o_, h=h, q0=q0, qn=qn: e.dma_start(out=self.oT_d[0, h * 128:(h + 1) * 128, q0:q0 + qn], in_=o_[:, 0:qn]), [ob_], [self.oT_b])
        k.es = k.es0
        k.barrier()

    def qk_prep(self, l, b, c0, G, gqk, gqkb, outs):
        k = self.k; inp = self.inp; T, NT, NLT, L = self.T, self.NT, self.NLT, self.L
        W = G * 64
        xq = [k.sb(f"xq{i}", [128, W]) for i in range(2)]
        junk, junkb = k.sb("qjunk", [128, W])
        t2, t2b = k.sb("qt2", [128, W])
        xb16 = [k.sb(f"xb16{i}", [128, W], BF16) for i in range(2)]
        ssq = [k.sb(f"ssq{i}", [128, G]) for i in range(2)]
        cs = [k.sb(f"cs{i}", [128, 64]) for i in range(2)]
        sn = [k.sb(f"sn{i}", [128, 64]) for i in range(2)]
        for tt in range(NT):
            x_, xb_ = xq[tt % 2]; s_, sb_ = ssq[tt % 2]; xo, xob = xb16[tt % 2]
            t0 = tt * 128
            k.dma("sp", lambda e, x_=x_, t0=t0: e.dma_start(out=x_[:], in_=self.proj_d[0, t0:t0 + 128, c0:c0 + W]), [self.proj_b], [xb_])
            k.op("act", lambda e, x_=x_: e.activation(out=junk[:], in_=x_[:], func=AF.Square), [xb_], [junkb])
            k.op("dve", lambda e, s_=s_: e.tensor_reduce(out=s_[:], in_=junk[:].rearrange("p (g c) -> p g c", g=G), axis=AX.X, op=ALU.add), [junkb], [sb_])
            self.rstd(s_[:], sb_, 64)
            x3 = x_[:].rearrange("p (g c) -> p g c", g=G)
            k.op("dve", lambda e, x3=x3, s_=s_: e.tensor_tensor(out=x3, in0=x3, in1=s_[:].unsqueeze(2).to_broadcast([128, G, 64]), op=ALU.mult), [xb_, sb_], [xb_])
            if tt < NLT:
                k.op("pool", lambda e, x3=x3, xo=xo: e.tensor_tensor(out=xo[:].rearrange("p (g c) -> p g c", g=G), in0=x3, in1=gqk[:, 0:G, :], op=ALU.mult), [xb_, gqkb], [xob])
            else:
                c_, cb_ = cs[tt % 2]; n_, nb_ = sn[tt % 2]
                p0 = t0 - L
                k.dma("pool", lambda e, c_=c_, p0=p0: e.dma_start(out=c_[:], in_=inp["rope_cos"][p0:p0 + 128, :]), [], [cb_])
                k.dma("pool", lambda e, n_=n_, p0=p0: e.dma_start(out=n_[:], in_=inp["rope_sin"][p0:p0 + 128, :]), [], [nb_])
                k.op("pool", lambda e, x3=x3: e.tensor_tensor(out=x3, in0=x3, in1=gqk[:, 0:G, :], op=ALU.mult), [xb_, gqkb], [xb_])
                x5 = x_[:].rearrange("p (g a s q) -> p g a s q", g=G, a=2, s=2)
                t5 = t2[:].rearrange("p (g a s q) -> p g a s q", g=G, a=2, s=2)
                n4 = n_[:].rearrange("p (a s q) -> p a s q", a=2, s=2)
                for s in range(2):
                    k.op("dve", lambda e, s=s, x5=x5, t5=t5, n4=n4: e.tensor_tensor(out=t5[:, :, :, s, :], in0=x5[:, :, :, 1 - s, :], in1=n4[:, :, s, :].unsqueeze(1).to_broadcast([128, G, 2, 16]), op=ALU.mult), [xb_, nb_], [t2b])
                k.op("pool", lambda e, x3=x3, c_=c_: e.tensor_tensor(out=x3, in0=x3, in1=c_[:].unsqueeze(1).to_broadcast([128, G, 64]), op=ALU.mult), [xb_, cb_], [xb_])
                k.op("dve", lambda e, x_=x_, xo=xo: e.tensor_tensor(out=xo[:], in0=x_[:], in1=t2[:], op=ALU.add), [xb_, t2b], [xob])
            for (dst, dstb, g0, npair, *wd) in outs:
                wd = wd[0] if wd else 2
                pt, ptb = self.psT[self.ev]
                for j in range(npair):
                    cc = (g0 + wd * j) * 64
                    k.op("pe", lambda e, pt=pt, j=j, cc=cc, xo=xo, wd=wd: e.transpose(out=pt[0:64 * wd, j, :], in_=xo[:, cc:cc + 64 * wd], identity=self.identb[:]), [xob, self.identb_b], [ptb])
                self.evac(dst[0:64 * wd, 0:npair, t0:t0 + 128], pt[0:64 * wd, 0:npair, :], [ptb], [dstb])

    def phase_B(self, l, b):
        k = self.k; inp = self.inp; NB, T, NT, NLT, L, S = self.NB, self.T, self.NT, self.NLT, self.L, self.S
        k.barrier()
        with ExitStack() as es:
            k.es = es
            self.psum_std()
            qT, qTb = k.sb("qTB", [64, 8, T], BF16)
            kT, kTb = k.sb("kTB", [64, 2, T], BF16)
            vB, vBb = k.sb("vB", [128, NT, 128], BF16)
            onesb, onesbb = k.sb("onesb", [128, 64], BF16)
            k.op("pool", lambda e: e.memset(onesb[:], 1.0), [], [onesbb])
            mlo, mlob = k.sb("mlo", [128, 128], BF16); mhi, mhib = k.sb("mhi", [128, 128], BF16)
            mf, mfb = k.sb("mf", [128, 128])
            k.dma("sp", lambda e: e.dma_start(out=mf[:], in_=inp["mask_lo"][:, :]), [], [mfb])
            k.op("dve", lambda e: e.tensor_copy(out=mlo[:], in_=mf[:]), [mfb], [mlob])
            k.dma("sp", lambda e: e.dma_start(out=mf[:], in_=inp["mask_hi"][:, :]), [mlob], [mfb])
            k.op("dve", lambda e: e.tensor_copy(out=mhi[:], in_=mf[:]), [mfb], [mhib])
            sk, skb = k.sb("sk", [128, 8])
            k.dma("sp", lambda e: e.dma_start(out=sk[:], in_=inp["wg_sink"][l, :].partition_broadcast(128)), [], [skb])
            k.op("act", lambda e: e.activation(out=sk[:], in_=sk[:], func=AF.Exp), [skb], [skb])
            gqk, gqkb = k.sb("gqkB", [128, 10, 64])
            for g in range(10):
                k.dma("pool", lambda e, g=g: e.dma_start(out=gqk[:, g, :], in_=inp["qk_norm_g"][l, 2 if g < 8 else 3, :].partition_broadcast(128)), [], [gqkb])
            self.qk_prep(l, b, C_QB, 10, gqk, gqkb, [(qT, qTb, 0, 8, 1), (kT, kTb, 8, 2, 1)])
            vf = [k.sb(f"vfB{i}", [128, 128]) for i in range(2)]
            for tt in range(NT):
                v_, vb_ = vf[tt % 2]
                k.dma("sp", lambda e, v_=v_, tt=tt: e.dma_start(out=v_[:], in_=self.proj_d[0, tt * 128:(tt + 1) * 128, C_VB:C_VB + 128]), [self.proj_b], [vb_])
                k.op("pool", lambda e, v_=v_, tt=tt: e.tensor_copy(out=vB[:, tt, :], in_=v_[:]), [vb_], [vBb])
            pT = [k.sb(f"pTB{i}", [128, 512], BF16) for i in range(3)]
            tot, totb = k.sb("totB", [64, 512])
            ob = [k.sb(f"obB{i}", [64, 512], BF16) for i in range(2)]
            n = 0; nob = 0
            for v in range(2):
                for tt in range(NT):
                    keys = [(kt, None) for kt in range(NLT)]
                    if tt >= NLT:
                        if tt - 1 >= NLT:
                            keys.append((tt - 1, (mlo, mlob)))
                        keys.append((tt, None))
                        if tt + 1 < NT:
                            keys.append((tt + 1, (mhi, mhib)))
                    psO, psOb = self.psA[2 + (nob % 2)]; psS, psSb = self.psA[4 + (nob % 2)]
                    t0 = tt * 128
                    for ki, (kt, msk) in enumerate(keys):
                        ps, psb = self.psA[n % 2]; p_, pb_ = pT[n % 3]; n += 1
                        first = (ki == 0); last = (ki == len(keys) - 1)
                        k.op("pe", lambda e, ps=ps, kt=kt, v=v, t0=t0: e.matmul(ps[:, :].rearrange("p (g q) -> p g q", g=4), lhsT=kT[0:64, v, kt * 128:(kt + 1) * 128], rhs=qT[0:64, 4 * v:4 * v + 4, t0:t0 + 128], start=True, stop=True), [kTb, qTb], [psb])
                        k.op("act", lambda e, ps=ps, p_=p_: e.activation(out=p_[:], in_=ps[:], func=AF.Exp, scale=0.125), [psb], [pb_])
                        if msk is not None:
                            k.op("dve", lambda e, p_=p_, msk=msk: e.tensor_tensor(out=p_[:].rearrange("p (g q) -> p g q", g=4), in0=p_[:].rearrange("p (g q) -> p g q", g=4), in1=msk[0][:].unsqueeze(1).to_broadcast([128, 4, 128]), op=ALU.mult), [pb_, msk[1]], [pb_])
                        k.op("pe", lambda e, psO=psO, kt=kt, v=v, p_=p_, first=first, last=last: e.matmul(psO[0:64, :], lhsT=vB[:, kt, 64 * v:64 * v + 64], rhs=p_[:], start=first, stop=last), [vBb, pb_], [psOb])
                        k.op("pe", lambda e, psS=psS, p_=p_, first=first, last=last: e.matmul(psS[0:64, :], lhsT=onesb[:], rhs=p_[:], start=first, stop=last), [onesbb, pb_], [psSb])
                    k.op("dve", lambda e, psS=psS, v=v: e.tensor_tensor(out=tot[:].rearrange("p (g q) -> p g q", g=4), in0=psS[0:64, :].rearrange("p (g q) -> p g q", g=4), in1=sk[0:64, 4 * v:4 * v + 4].unsqueeze(2).to_broadcast([64, 4, 128]), op=ALU.add), [psSb, skb], [totb])
                    k.op("dve", lambda e: e.reciprocal(out=tot[:], in_=tot[:]), [totb], [totb])
                    o_, ob_ = ob[nob % 2]; nob += 1
                    k.op("dve", lambda e, o_=o_, psO=psO: e.tensor_tensor(out=o_[:], in0=psO[0:64, :], in1=tot[:], op=ALU.mult), [psOb, totb], [ob_])
                    k.dma("sp", lambda e, o_=o_, v=v, t0=t0: e.dma_start(out=self.oT_d[0, 512 + 256 * v:512 + 256 * v + 256, t0:t0 + 128].rearrange("(g d) t -> d g t", g=4), in_=o_[:].rearrange("p (g q) -> p g q", g=4)), [ob_], [self.oT_b])
        k.es = k.es0
        k.barrier()

    def phase_C(self, l, b):
        try:
            self._phase_C(l, b)
        except StopPhase:
            pass
        self.k.es = self.k.es0
        self.k.barrier()

    def cstop(self, n):
        return self.cfg.get("c_stop") == n

    def _phase_C(self, l, b):
        k = self.k; inp = self.inp; NB, T, NT, NLT, L, S = self.NB, self.T, self.NT, self.NLT, self.L, self.S
        k.barrier()
        with ExitStack() as es:
            k.es = es
            banks = [k.ps(f"pc{i}", [128, 512]) for i in range(6)]
            slots = [(banks[i][0][:, 0:128], banks[i][1]) for i in range(6)]
            self.psT = [k.ps(f"psT{i}", [128, 8, 128], BF16) for i in range(2)]
            cnt = [0]
            def PS():
                sl = slots[cnt[0] % 6]; cnt[0] += 1
                return sl
            rings = {}
            def W(name, shape=(128, 128), depth=2):
                if name not in rings:
                    rings[name] = [[k.sb(f"{name}{i}", list(shape)) for i in range(depth)], 0]
                r = rings[name]; r[1] += 1
                return r[0][r[1] % depth]
            ident = self.ident; identb_ = self.ident_b
            gm, gmb = k.sb("gm", [128, 8, 128])
            k.dma("sp", lambda e: e.dma_start(out=gm[:], in_=inp["gmasks"].rearrange("m p f -> p m f")), [], [gmb])
            LE, GE, LT, GT, BD, SEL0, SEL1 = (gm[:, i, :] for i in range(7))
            bias_le, blb = k.sb("bias_le", [128, 128]); bias_ge, bgb = k.sb("bias_ge", [128, 128])
            k.op("dve", lambda e: e.tensor_scalar(out=bias_le[:], in0=LE, scalar1=-1.0, scalar2=30000.0, op0=ALU.add, op1=ALU.mult), [gmb], [blb])
            k.op("dve", lambda e: e.tensor_scalar(out=bias_ge[:], in0=GE, scalar1=-1.0, scalar2=30000.0, op0=ALU.add, op1=ALU.mult), [gmb], [bgb])
            onesf, onesfb = k.sb("onesfC", [128, 128])
            k.op("pool", lambda e: e.memset(onesf[:], 1.0), [], [onesfb])
            araw, arawb = k.sb("araw", [128, NT, 8]); braw, brawb = k.sb("braw", [128, NT, 8])
            g3, g3b = k.sb("g3", [128, NT, 8]); ng3, ng3b = k.sb("ng3", [128, NT, 8])
            be3, be3b = k.sb("be3", [128, NT, 8]); nbe3, nbe3b = k.sb("nbe3", [128, NT, 8])
            tmp3, tmp3b = k.sb("tmp3", [128, NT, 8])
            dtb, dtbb = k.sb("dtb", [128, 8]); nea, neab = k.sb("nea", [128, 8])
            k.dma("sp", lambda e: e.dma_start(out=araw[:], in_=self.proj_d[0, :, C_AC:C_AC + 8].rearrange("(n p) c -> p n c", p=128)), [self.proj_b], [arawb])
            k.dma("sp", lambda e: e.dma_start(out=braw[:], in_=self.proj_d[0, :, C_BC:C_BC + 8].rearrange("(n p) c -> p n c", p=128)), [self.proj_b], [brawb])
            k.dma("pool", lambda e: e.dma_start(out=dtb[:], in_=inp["gd_dt_bias"][l].rearrange("a b -> (a b)").partition_broadcast(128)), [], [dtbb])
            k.dma("pool", lambda e: e.dma_start(out=nea[:], in_=inp["gd_a_log"][l].rearrange("a b -> (a b)").partition_broadcast(128)), [], [neab])
            k.op("act", lambda e: e.activation(out=nea[:], in_=nea[:], func=AF.Exp), [neab], [neab])
            k.op("dve", lambda e: e.tensor_scalar(out=nea[:], in0=nea[:], scalar1=-1.0, scalar2=None, op0=ALU.mult), [neab], [neab])
            k.op("dve", lambda e: e.tensor_tensor(out=araw[:], in0=araw[:], in1=dtb[:].unsqueeze(1).to_broadcast([128, NT, 8]), op=ALU.add), [arawb, dtbb], [arawb])
            k.op("act", lambda e: e.activation(out=tmp3[:], in_=araw[:], func=AF.Abs), [arawb], [tmp3b])
            k.op("act", lambda e: e.activation(out=tmp3[:], in_=tmp3[:], func=AF.Exp, scale=-1.0), [tmp3b], [tmp3b])
            k.op("act", lambda e: e.activation(out=tmp3[:], in_=tmp3[:], func=AF.Ln, bias=1.0), [tmp3b], [tmp3b])
            k.op("dve", lambda e: e.scalar_tensor_tensor(out=tmp3[:], in0=araw[:], scalar=0.0, in1=tmp3[:], op0=ALU.max, op1=ALU.add), [arawb, tmp3b], [tmp3b])
            k.op("dve", lambda e: e.tensor_tensor(out=g3[:], in0=tmp3[:], in1=nea[:].unsqueeze(1).to_broadcast([128, NT, 8]), op=ALU.mult), [tmp3b, neab], [g3b])
            k.op("dve", lambda e: e.tensor_scalar(out=ng3[:], in0=g3[:], scalar1=-1.0, scalar2=None, op0=ALU.mult), [g3b], [ng3b])
            k.op("act", lambda e: e.activation(out=be3[:], in_=braw[:], func=AF.Sigmoid), [brawb], [be3b])
            k.op("dve", lambda e: e.tensor_scalar(out=nbe3[:], in0=be3[:], scalar1=-1.0, scalar2=None, op0=ALU.mult), [be3b], [nbe3b])
            if self.cstop(1):
                return
            xin, xinb = k.sb("xin", [128, T]); ybuf, ybufb = k.sb("ybuf", [128, T])
            qT, qTb = k.sb("qTC", [128, T]); kT, kTb = k.sb("kTC", [128, T])
            k_tm, k_tmb = k.sb("k_tm", [128, NT, 128]); v_tm, v_tmb = k.sb("v_tm", [128, NT, 128])
            o_acc, o_accb = k.sb("o_acc", [128, NT, 128])
            S_, Sb = k.sb("Sstate", [128, 128])
            cw, cwb = k.sb("cw", [128, 5])
            gng, gngb = k.sb("gng", [128, 128])
            k.dma("pool", lambda e: e.dma_start(out=gng[:], in_=inp["gd_norm_g"][l, :].partition_broadcast(128)), [], [gngb])
            segs = [(0, L), (L, T)]
            for h in range(4):
                for part, (dst, dstb) in enumerate(((qT, qTb), (kT, kTb), (ybuf, ybufb))):
                    ch0 = (part * 4 + h) * 128
                    k.dma("sp", lambda e, ch0=ch0: e.dma_start(out=xin[:], in_=self.qkvT_d[0, ch0:ch0 + 128, :]), [self.qkvT_b], [xinb])
                    k.dma("pool", lambda e, ch0=ch0: e.dma_start(out=cw[:], in_=inp["gd_conv_w"][l, :, ch0:ch0 + 128].rearrange("j c -> c j"), allow_slow_non_contiguous=True), [], [cwb])
                    k.op("dve", lambda e, dst=dst: e.tensor_scalar(out=dst[:], in0=xin[:], scalar1=cw[:, 2:3], scalar2=None, op0=ALU.mult), [xinb, cwb], [dstb])
                    for j in (0, 1, 3, 4):
                        d = j - 2
                        for (a, z) in segs:
                            lo, hi = (a, z - d) if d > 0 else (a - d, z)
                            k.op("dve", lambda e, dst=dst, lo=lo, hi=hi, d=d, j=j: e.scalar_tensor_tensor(out=dst[:, lo:hi], in0=xin[:, lo + d:hi + d], scalar=cw[:, j:j + 1], in1=dst[:, lo:hi], op0=ALU.mult, op1=ALU.add), [xinb, cwb, dstb], [dstb])
                    k.op("act", lambda e, dst=dst: e.activation(out=dst[:], in_=dst[:], func=AF.Silu), [dstb], [dstb])
                    if self.cstop(5):
                        return
                    if part < 2:
                        k.op("act", lambda e, dst=dst: e.activation(out=xin[:], in_=dst[:], func=AF.Square), [dstb], [xinb])
                        for c0 in range(0, T, 512):
                            cn = min(512, T - c0)
                            bi = (c0 // 512) % 2
                            ps = banks[bi][0]; bbufs = [banks[bi][1]]
                            k.op("pe", lambda e, ps=ps, c0=c0, cn=cn: e.matmul(ps[:, 0:cn], lhsT=onesf[:], rhs=xin[:, c0:c0 + cn], start=True, stop=True), [onesfb, xinb], bbufs)
                            k.op("act", lambda e, ps=ps, c0=c0, cn=cn: e.activation(out=xin[:, c0:c0 + cn], in_=ps[:, 0:cn], func=AF.Ln, bias=EPS), bbufs + [xinb], [xinb])
                        k.op("act", lambda e: e.activation(out=xin[:], in_=xin[:], func=AF.Exp, scale=-0.5), [xinb], [xinb])
                        sc = (128.0 ** -0.5) if part == 0 else 1.0
                        k.op("dve", lambda e, dst=dst, sc=sc: e.scalar_tensor_tensor(out=dst[:], in0=dst[:], scalar=sc, in1=xin[:], op0=ALU.mult, op1=ALU.mult), [dstb, xinb], [dstb])
                    if part >= 1:
                        if self.cstop(60 + part):
                            return
                        tm, tmb = (k_tm, k_tmb) if part == 1 else (v_tm, v_tmb)
                        for tt in range(NT):
                            ps, psb = PS()
                            k.op("pe", lambda e, ps=ps, dst=dst, tt=tt: e.matmul(ps, lhsT=dst[:, tt * 128:(tt + 1) * 128], rhs=ident[:], start=True, stop=True), [dstb, identb_], [psb])
                            self.evac(tm[:, tt, :], ps, [psb], [tmb])
                        if self.cstop(70 + part):
                            return
                if self.cstop(2):
                    return
                k.op("pool", lambda e: e.memset(o_acc[:], 0.0), [], [o_accb])
                for d in range(2):
                    c = d * 4 + h
                    Linc = LE if d == 0 else GE
                    bias_i, bias_ib = (bias_ge, bgb) if d == 0 else (bias_le, blb)
                    St = GT if d == 0 else LT
                    bias_t, bias_tb = (bias_le, blb) if d == 0 else (bias_ge, bgb)
                    k.op("pool", lambda e: e.memset(S_[:], 0.0), [], [Sb])
                    order = list(range(NT)) if d == 0 else (list(range(NLT - 1, -1, -1)) + list(range(NT - 1, NLT - 1, -1)))
                    for tt in order:
                        t0 = tt * 128
                        gcol = g3[:, tt, c:c + 1]; ngcol = ng3[:, tt, c:c + 1]
                        G, Gb = W("G"); nG, nGb = W("nG")
                        k.op("dve", lambda e, G=G, gcol=gcol, Linc=Linc: e.tensor_scalar(out=G[:], in0=Linc, scalar1=gcol, scalar2=None, op0=ALU.mult), [gmb, g3b], [Gb])
                        k.op("pool", lambda e, nG=nG, ngcol=ngcol, Linc=Linc: e.tensor_scalar(out=nG[:], in0=Linc, scalar1=ngcol, scalar2=None, op0=ALU.mult), [gmb, ng3b], [nGb])
                        pD, pDb = PS(); pDT, pDTb = PS(); pg, pgb = PS()
                        k.op("pe", lambda e, pD=pD, G=G: e.matmul(pD, lhsT=G[:], rhs=onesf[:], start=True, stop=False), [Gb, onesfb], [pDb])
                        k.op("pe", lambda e, pD=pD, nG=nG: e.matmul(pD, lhsT=onesf[:], rhs=nG[:], start=False, stop=True), [nGb, onesfb], [pDb])
                        k.op("pe", lambda e, pDT=pDT, G=G: e.matmul(pDT, lhsT=onesf[:], rhs=G[:], start=True, stop=False), [Gb, onesfb], [pDTb])
                        k.op("pe", lambda e, pDT=pDT, nG=nG: e.matmul(pDT, lhsT=nG[:], rhs=onesf[:], start=False, stop=True), [nGb, onesfb], [pDTb])
                        for ci, msk in enumerate((Linc, BD, SEL0, SEL1)):
                            k.op("pe", lambda e, pg=pg, ci=ci, msk=msk, gcol=gcol: e.matmul(pg[:, ci:ci + 1], lhsT=msk, rhs=gcol, start=True, stop=True), [gmb, g3b], [pgb])
                        sc4, sc4b = W("sc4", (128, 8))
                        k.op("dve", lambda e, sc4=sc4, pg=pg: e.tensor_copy(out=sc4[:, 7:8], in_=pg[:, 0:1]), [pgb], [sc4b])
                        k.op("act", lambda e, sc4=sc4, pg=pg: e.activation(out=sc4[:, 0:1], in_=pg[:, 0:1], func=AF.Exp), [pgb], [sc4b])
                        k.op("dve", lambda e, sc4=sc4, pg=pg: e.tensor_tensor(out=sc4[:, 1:2], in0=pg[:, 1:2], in1=sc4[:, 7:8], op=ALU.subtract), [pgb, sc4b], [sc4b])
                        k.op("act", lambda e, sc4=sc4: e.activation(out=sc4[:, 1:2], in_=sc4[:, 1:2], func=AF.Exp), [sc4b], [sc4b])
                        k.op("act", lambda e, sc4=sc4, pg=pg: e.activation(out=sc4[:, 2:4], in_=pg[:, 2:4], func=AF.Exp), [pgb], [sc4b])
                        k.op("dve", lambda e, sc4=sc4, tt=tt, c=c: e.tensor_tensor(out=sc4[:, 4:5], in0=sc4[:, 0:1], in1=be3[:, tt, c:c + 1], op=ALU.mult), [sc4b, be3b], [sc4b])
                        k.op("dve", lambda e, sc4=sc4: e.tensor_tensor(out=sc4[:, 5:6], in0=sc4[:, 1:2], in1=SEL0[:, 0:1], op=ALU.mult), [sc4b, gmb], [sc4b])
                        k.op("dve", lambda e, sc4=sc4: e.tensor_tensor(out=sc4[:, 6:7], in0=sc4[:, 1:2], in1=SEL1[:, 0:1], op=ALU.mult), [sc4b, gmb], [sc4b])
                        dec, decb = W("dec"); decS, decSb = W("decS"); decT, decTb = W("decT")
                        k.op("dve", lambda e, dec=dec, pD=pD, bias_i=bias_i: e.tensor_tensor(out=dec[:], in0=pD, in1=bias_i[:], op=ALU.add), [pDb, bias_ib], [decb])
                        k.op("act", lambda e, dec=dec: e.activation(out=dec[:], in_=dec[:], func=AF.Exp), [decb], [decb])
                        k.op("pool", lambda e, dec=dec, decS=decS, St=St: e.tensor_tensor(out=decS[:], in0=dec[:], in1=St, op=ALU.mult), [decb, gmb], [decSb])
                        k.op("dve", lambda e, decT=decT, pDT=pDT, bias_t=bias_t: e.tensor_tensor(out=decT[:], in0=pDT, in1=bias_t[:], op=ALU.add), [pDTb, bias_tb], [decTb])
                        k.op("act", lambda e, decT=decT: e.activation(out=decT[:], in_=decT[:], func=AF.Exp), [decTb], [decTb])
                        pKK, pKKb = PS(); pQK, pQKb = PS()
                        k.op("pe", lambda e, pKK=pKK, t0=t0: e.matmul(pKK, lhsT=kT[:, t0:t0 + 128], rhs=kT[:, t0:t0 + 128], start=True, stop=True), [kTb], [pKKb])
                        k.op("pe", lambda e, pQK=pQK, t0=t0: e.matmul(pQK, lhsT=kT[:, t0:t0 + 128], rhs=qT[:, t0:t0 + 128], start=True, stop=True), [kTb, qTb], [pQKb])
                        Q, Qb = W("Q", depth=3); P, Pb = W("P", depth=3); TT, TTb = W("TT"); qkmT, qkmTb = W("qkmT")
                        k.op("dve", lambda e, Q=Q, pKK=pKK, decS=decS, tt=tt, c=c: e.scalar_tensor_tensor(out=Q[:], in0=pKK, scalar=nbe3[:, tt, c:c + 1], in1=decS[:], op0=ALU.mult, op1=ALU.mult), [pKKb, nbe3b, decSb], [Qb])
                        k.op("dve", lambda e, qkmT=qkmT, pQK=pQK, decT=decT: e.tensor_tensor(out=qkmT[:], in0=pQK, in1=decT[:], op=ALU.mult), [pQKb, decTb], [qkmTb])
                        pP, pPb = PS()
                        k.op("pe", lambda e, pP=pP, Q=Q: e.matmul(pP, lhsT=Q[:], rhs=ident[:], start=True, stop=True), [Qb, identb_], [pPb])
                        k.op("act", lambda e, P=P, pP=pP: e.activation(out=P[:], in_=pP, func=AF.Copy), [pPb], [Pb])
                        k.op("dve", lambda e, TT=TT, pP=pP: e.tensor_tensor(out=TT[:], in0=pP, in1=ident[:], op=ALU.add), [pPb, identb_], [TTb])
                        for lev in range(5):
                            Qn, Qnb = W("Q", depth=3); pQn, pQnb = PS()
                            k.op("pe", lambda e, pQn=pQn, P=P, Q=Q: e.matmul(pQn, lhsT=P[:], rhs=Q[:], start=True, stop=True), [Pb, Qb], [pQnb])
                            if lev < 4:
                                Pn, Pnb = W("P", depth=3); pPn, pPnb = PS()
                                k.op("pe", lambda e, pPn=pPn, P=P, Q=Q: e.matmul(pPn, lhsT=Q[:], rhs=P[:], start=True, stop=True), [Pb, Qb], [pPnb])
                                k.op("act", lambda e, Pn=Pn, pPn=pPn: e.activation(out=Pn[:], in_=pPn, func=AF.Copy), [pPnb], [Pnb])
                            k.op("dve", lambda e, Qn=Qn, pQn=pQn: e.tensor_copy(out=Qn[:], in_=pQn), [pQnb], [Qnb])
                            pU, pUb = PS()
                            k.op("pe", lambda e, pU=pU, Qn=Qn, TT=TT: e.matmul(pU, lhsT=Qn[:], rhs=TT[:], start=True, stop=True), [Qnb, TTb], [pUb])
                            k.op("dve", lambda e, TT=TT, pU=pU: e.tensor_tensor(out=TT[:], in0=TT[:], in1=pU, op=ALU.add), [TTb, pUb], [TTb])
                            Q, Qb = Qn, Qnb
                            if lev < 4:
                                P, Pb = Pn, Pnb
                        if self.cstop(3):
                            return
                        vb, vbb = W("vb"); kbg, kbgb = W("kbg"); kd0, kd0b = W("kd0"); kd1, kd1b = W("kd1")
                        k.op("pool", lambda e, vb=vb, tt=tt, c=c: e.tensor_scalar(out=vb[:], in0=v_tm[:, tt, :], scalar1=be3[:, tt, c:c + 1], scalar2=None, op0=ALU.mult), [v_tmb, be3b], [vbb])
                        k.op("pool", lambda e, kbg=kbg, tt=tt, sc4=sc4: e.tensor_scalar(out=kbg[:], in0=k_tm[:, tt, :], scalar1=sc4[:, 4:5], scalar2=None, op0=ALU.mult), [k_tmb, sc4b], [kbgb])
                        k.op("pool", lambda e, kd0=kd0, tt=tt, sc4=sc4: e.tensor_scalar(out=kd0[:], in0=k_tm[:, tt, :], scalar1=sc4[:, 5:6], scalar2=None, op0=ALU.mult), [k_tmb, sc4b], [kd0b])
                        k.op("pool", lambda e, kd1=kd1, tt=tt, sc4=sc4: e.tensor_scalar(out=kd1[:], in0=k_tm[:, tt, :], scalar1=sc4[:, 6:7], scalar2=None, op0=ALU.mult), [k_tmb, sc4b], [kd1b])
                        pu, pub = PS(); pw, pwb = PS()
                        k.op("pe", lambda e, pu=pu, TT=TT, vb=vb: e.matmul(pu, lhsT=TT[:], rhs=vb[:], start=True, stop=True), [TTb, vbb], [pub])
                        k.op("pe", lambda e, pw=pw, TT=TT, kbg=kbg: e.matmul(pw, lhsT=kbg[:], rhs=TT[:], start=True, stop=True), [TTb, kbgb], [pwb])
                        u, ub = W("u"); wT, wTb = W("wT")
                        k.op("act", lambda e, u=u, pu=pu: e.activation(out=u[:], in_=pu, func=AF.Copy), [pub], [ub])
                        k.op("dve", lambda e, wT=wT, pw=pw: e.tensor_copy(out=wT[:], in_=pw), [pwb], [wTb])
                        for ch in ((0, 1) if d == 0 else (1, 0)):
                            kd, kdb = (kd0, kd0b) if ch == 0 else (kd1, kd1b)
                            cm = (SEL0 if ch == 0 else SEL1)[:, 0:1]
                            pwS, pwSb = PS(); pqS, pqSb = PS(); po1, po1b = PS(); pSn, pSnb = PS()
                            k.op("pe", lambda e, pwS=pwS, wT=wT: e.matmul(pwS, lhsT=wT[:], rhs=S_[:], start=True, stop=True), [wTb, Sb], [pwSb])
                            k.op("pe", lambda e, pqS=pqS, t0=t0: e.matmul(pqS, lhsT=qT[:, t0:t0 + 128], rhs=S_[:], start=True, stop=True), [qTb, Sb], [pqSb])
                            vn, vnb = W("vn"); tq, tqb = W("tq")
                            k.op("dve", lambda e, vn=vn, u=u, pwS=pwS: e.tensor_tensor(out=vn[:], in0=u[:], in1=pwS, op=ALU.subtract), [ub, pwSb], [vnb])
                            k.op("pe", lambda e, po1=po1, qkmT=qkmT, vn=vn: e.matmul(po1, lhsT=qkmT[:], rhs=vn[:], start=True, stop=True), [qkmTb, vnb], [po1b])
                            k.op("pe", lambda e, pSn=pSn, kd=kd, vn=vn: e.matmul(pSn, lhsT=kd[:], rhs=vn[:], start=True, stop=True), [kdb, vnb], [pSnb])
                            k.op("act", lambda e, tq=tq, pqS=pqS, sc4=sc4: e.activation(out=tq[:], in_=pqS, func=AF.Copy, scale=sc4[:, 0:1]), [pqSb, sc4b], [tqb])
                            k.op("dve", lambda e, tq=tq, po1=po1: e.tensor_tensor(out=tq[:], in0=tq[:], in1=po1, op=ALU.add), [tqb, po1b], [tqb])
                            k.op("dve", lambda e, tq=tq, cm=cm, tt=tt: e.scalar_tensor_tensor(out=o_acc[:, tt, :], in0=tq[:], scalar=cm, in1=o_acc[:, tt, :], op0=ALU.mult, op1=ALU.add), [tqb, gmb, o_accb], [o_accb])
                            k.op("dve", lambda e, pSn=pSn, sc4=sc4, ch=ch: e.scalar_tensor_tensor(out=S_[:], in0=S_[:], scalar=sc4[:, 2 + ch:3 + ch], in1=pSn, op0=ALU.mult, op1=ALU.add), [Sb, sc4b, pSnb], [Sb])
                if self.cstop(4):
                    return
                zt = xin[:, 0:NT * 128].rearrange("p (n c) -> p n c", c=128)
                k.dma("sp", lambda e, h=h: e.dma_start(out=zt, in_=self.proj_d[0, :, C_ZC + h * 128:C_ZC + (h + 1) * 128].rearrange("(n p) c -> p n c", p=128)), [self.proj_b], [xinb])
                k.op("act", lambda e: e.activation(out=zt, in_=zt, func=AF.Silu), [xinb], [xinb])
                sq3 = ybuf[:, 0:NT * 128].rearrange("p (n c) -> p n c", c=128)
                ssn, ssnb = W("ssn", (128, NT), depth=1)
                k.op("act", lambda e: e.activation(out=sq3, in_=o_acc[:], func=AF.Square), [o_accb], [ybufb])
                k.op("dve", lambda e, ssn=ssn: e.tensor_reduce(out=ssn[:], in_=sq3, axis=AX.X, op=ALU.add), [ybufb], [ssnb])
                self.rstd(ssn[:], ssnb, 128)
                k.op("dve", lambda e, ssn=ssn: e.tensor_tensor(out=o_acc[:], in0=o_acc[:], in1=ssn[:].unsqueeze(2).to_broadcast([128, NT, 128]), op=ALU.mult), [o_accb, ssnb], [o_accb])
                k.op("pool", lambda e: e.tensor_tensor(out=o_acc[:], in0=o_acc[:], in1=gng[:].unsqueeze(1).to_broadcast([128, NT, 128]), op=ALU.mult), [o_accb, gngb], [o_accb])
                ob16 = kT[:].bitcast(BF16)[:, 0:NT * 128].rearrange("p (n c) -> p n c", c=128)
                k.op("dve", lambda e, ob16=ob16: e.tensor_tensor(out=ob16, in0=o_acc[:], in1=zt, op=ALU.mult), [o_accb, xinb], [kTb])
                oT16 = qT[:].bitcast(BF16)[:, 0:T]
                for t8 in range(0, NT, 8):
                    n8 = min(8, NT - t8)
                    pt, ptb = self.psT[(t8 // 8) % 2]
                    for j in range(n8):
                        k.op("pe", lambda e, pt=pt, j=j, t8=t8, ob16=ob16: e.transpose(out=pt[:, j, :], in_=ob16[:, t8 + j, :], identity=self.identb[:]), [kTb, self.identb_b], [ptb])
                    self.evac(oT16[:, t8 * 128:(t8 + n8) * 128].rearrange("p (n c) -> p n c", c=128), pt[:, 0:n8, :], [ptb], [qTb])
                k.dma("sp", lambda e, h=h, oT16=oT16: e.dma_start(out=self.oT_d[0, 1024 + h * 128:1024 + (h + 1) * 128, :], in_=oT16), [qTb], [self.oT_b])
        k.es = k.es0
        k.barrier()

    def load_w16(self, dst16, dstb, src_ap_fn, ncols, stage):
        k = self.k
        for i, c0 in enumerate(range(0, ncols, 256)):
            cw = min(256, ncols - c0)
            s_, sb_ = stage[i % len(stage)]
            k.dma("sp", lambda e, s_=s_, c0=c0, cw=cw: e.dma_start(out=s_[:, :, 0:cw], in_=src_ap_fn(c0, cw)), [], [sb_])
            k.op("pool", lambda e, s_=s_, c0=c0, cw=cw: e.tensor_copy(out=dst16[:, :, c0:c0 + cw], in_=s_[:, :, 0:cw]), [sb_], [dstb])

    def phase_M(self, l, b):
        k = self.k; inp = self.inp; NB, T, NT, NLT = self.NB, self.T, self.NT, self.NLT
        last = (l == self.DEPTH - 1)
        k.barrier()
        with ExitStack() as es:
            k.es = es
            self.psum_std()
            stage = [k.sb(f"stg{i}", [128, 8, 256]) for i in range(2)]
            wbr, wbrb = k.sb("wbr", [128, 12, D], BF16)
            wout, woutb = k.sb("wout", [128, 8, D], BF16)
            for r in range(3):
                for c in range(4):
                    pass
            st4 = [k.sb(f"stg4{i}", [128, 4, 256]) for i in range(2)]
            n = 0
            for r in range(3):
                for c0 in range(0, D, 256):
                    s_, sb_ = st4[n % 2]; n += 1
                    k.dma("sp", lambda e, s_=s_, r=r, c0=c0: e.dma_start(out=s_[:], in_=inp["w_branch"][l, r, :, c0:c0 + 256].rearrange("(c p) d -> p c d", p=128)), [], [sb_])
                    k.op("pool", lambda e, s_=s_, r=r, c0=c0: e.tensor_copy(out=wbr[:, 4 * r:4 * r + 4, c0:c0 + 256], in_=s_[:]), [sb_], [wbrb])
            self.load_w16(wout, woutb, lambda c0, cw: inp["w_out"][l, :, c0:c0 + cw].rearrange("(k p) c -> p k c", p=128), D, stage)
            bg, bgb = k.sb("bgate", [128, 3 * D])
            k.dma("sp", lambda e: e.dma_start(out=bg[:], in_=inp["b_gate"][l, :].partition_broadcast(128)), [], [bgb])
            gt1, gt1b = k.sb("gt1", [128, D])
            gl, glb = k.sb("gl", [128, 3 * D])
            oT = [k.sb(f"oTm{i}", [128, 12, 128], BF16) for i in range(2)]
            xt, xtb = k.sb("xm", [128, D]); mp, mpb = k.sb("mp", [128, D]); tmpm, tmpmb = k.sb("tmpm", [128, D])
            mp16, mp16b = k.sb("mp16", [128, D], BF16); mT, mTb = k.sb("mTm", [128, 8, 128], BF16)
            cur_kind = None
            for tt in range(NT):
                kind = "ctx" if tt < NLT else "lat"
                if kind == "ctx" and last:
                    continue
                if kind != cur_kind:
                    cur_kind = kind
                    r_ = NB if kind == "ctx" else b
                    k.dma("sp", lambda e, r_=r_: e.dma_start(out=gt1[:], in_=self.mod_d[r_, 2 * D:3 * D].partition_broadcast(128)), [self.mod_b], [gt1b])
                t0 = tt * 128
                o_, ob_ = oT[tt % 2]
                k.dma("sp", lambda e, o_=o_, t0=t0: e.dma_start(out=o_[:], in_=self.oT_d[0, :, t0:t0 + 128].rearrange("(c p) t -> p c t", p=128)), [self.oT_b], [ob_])
                k.dma("pool", lambda e, t0=t0: e.dma_start(out=gl[:], in_=self.proj_d[0, t0:t0 + 128, C_GL:C_GL + 3 * D]), [self.proj_b], [glb])
                k.dma("sp", lambda e, t0=t0: e.dma_start(out=xt[:], in_=self.xs[b, t0:t0 + 128, :]), [self.xs_b], [xtb])
                k.op("pool", lambda e: e.tensor_tensor(out=gl[:], in0=gl[:], in1=bg[:], op=ALU.add), [glb, bgb], [glb])
                k.op("act", lambda e: e.activation(out=gl[:], in_=gl[:], func=AF.Sigmoid), [glb], [glb])
                for r in range(3):
                    for hf in range(2):
                        ps, psb = self.psA[r * 2 + hf]
                        for c in range(4):
                            k.op("pe", lambda e, ps=ps, r=r, c=c, hf=hf, o_=o_: e.matmul(ps[:], lhsT=o_[:, 4 * r + c, :], rhs=wbr[:, 4 * r + c, hf * 512:(hf + 1) * 512], start=(c == 0), stop=(c == 3)), [ob_, wbrb], [psb])
                for hf in range(2):
                    sl = slice(hf * 512, (hf + 1) * 512)
                    for r in range(3):
                        ps, psb = self.psA[r * 2 + hf]
                        dst, dstb = (mp, mpb) if r == 0 else (tmpm, tmpmb)
                        k.op("dve", lambda e, ps=ps, dst=dst, r=r, sl=sl: e.tensor_tensor(out=dst[:, sl], in0=ps[:], in1=gl[:, r * D + sl.start:r * D + sl.stop], op=ALU.mult), [psb, glb], [dstb])
                        if r > 0:
                            k.op("pool", lambda e, sl=sl: e.tensor_tensor(out=mp[:, sl], in0=mp[:, sl], in1=tmpm[:, sl], op=ALU.add), [mpb, tmpmb], [mpb])
                k.op("act", lambda e: e.activation(out=mp16[:], in_=mp[:], func=AF.Copy), [mpb], [mp16b])
                pt, ptb = self.psT[tt % 2]
                for kk in range(KD):
                    k.op("pe", lambda e, pt=pt, kk=kk: e.transpose(out=pt[:, kk, :], in_=mp16[:, kk * 128:(kk + 1) * 128], identity=self.identb[:]), [mp16b, self.identb_b], [ptb])
                self.evac(mT[:], pt[:], [ptb], [mTb])
                for hf in range(2):
                    sl = slice(hf * 512, (hf + 1) * 512)
                    ps, psb = self.psA[hf]
                    for kk in range(KD):
                        k.op("pe", lambda e, ps=ps, kk=kk, sl=sl: e.matmul(ps[:], lhsT=mT[:, kk, :], rhs=wout[:, kk, sl], start=(kk == 0), stop=(kk == KD - 1)), [mTb, woutb], [psb])
                    k.op("dve", lambda e, ps=ps, sl=sl: e.tensor_tensor(out=tmpm[:, sl], in0=ps[:], in1=gt1[:, sl], op=ALU.mult), [psb, gt1b], [tmpmb])
                    k.op("pool", lambda e, sl=sl: e.tensor_tensor(out=xt[:, sl], in0=xt[:, sl], in1=tmpm[:, sl], op=ALU.add), [xtb, tmpmb], [xtb])
                k.dma("sp", lambda e, t0=t0: e.dma_start(out=self.xs[b, t0:t0 + 128, :], in_=xt[:]), [xtb], [self.xs_b])
        k.es = k.es0
        k.barrier()

    def phase_P(self, l, b):
        k = self.k; inp = self.inp; NB, T, NT, NLT = self.NB, self.T, self.NT, self.NLT
        last = (l == self.DEPTH - 1)
        k.barrier()
        with ExitStack() as es:
            k.es = es
            self.psum_std()
            stage = [k.sb(f"stgp{i}", [128, 8, 256]) for i in range(2)]
            wq, wqb = k.sb("wq", [128, 8, 2048], BF16)
            self.load_w16(wq, wqb, lambda c0, cw: inp["peer_wq"][l, :, c0:c0 + cw].rearrange("(k p) c -> p k c", p=128), 2048, stage)
            keysT, keysTb = k.sb("keysT", [128, 2, 128])
            kn, knb = k.sb("keysN", [128, 2, 128])
            k.dma("pool", lambda e: e.dma_start(out=kn[:], in_=inp["peer_keys"][l].rearrange("f n d -> n f d")), [], [knb])
            for hf in range(2):
                ps, psb = self.psA[hf]
                k.op("pe", lambda e, ps=ps, hf=hf: e.matmul(ps[:, 0:128], lhsT=kn[:, hf, :], rhs=self.ident[:], start=True, stop=True), [knb, self.ident_b], [psb])
                self.evac(keysT[:, hf, :], ps[:, 0:128], [psb], [keysTb])
            A2, A2b = k.sb("A2", [128, D]); g2, g2b = k.sb("g2", [128, D]); B2, B2b = k.sb("B2", [128, D]); gt2, gt2b = k.sb("gt2", [128, D])
            k.dma("pool", lambda e: e.dma_start(out=g2[:], in_=inp["norm2_g"][l, :].partition_broadcast(128)), [], [g2b])
            iot, iotb = k.sb("iota16", [128, 16])
            k.op("pool", lambda e: e.iota(iot[:], pattern=[[1, 16]], base=0, channel_multiplier=0, allow_small_or_imprecise_dtypes=True), [], [iotb])
            xt, xtb = k.sb("xp", [128, D]); junk, junkb = k.sb("junkp", [128, D]); ss, ssb = k.sb("ssp", [128, 1])
            h2, h2b = k.sb("h2", [128, D]); h16, h16b = k.sb("h16", [128, D], BF16); h2T, h2Tb = k.sb("h2T", [128, 8, 128], BF16)
            qTs, qTsb = k.sb("qTs", [128, 16, 128]); sc, scb = k.sb("sc", [128, 16, 128]); wk, wkb = k.sb("wk", [128, 16, 128])
            m16, m16b = k.sb("m16", [128, 16, 16]); ix, ixb = k.sb("ix", [128, 16, 16], U32); ixf, ixfb = k.sb("ixf", [128, 16, 16])
            cand, candb = k.sb("cand", [128, 8, 256])
            cwk = qTs[:].rearrange("p g n -> p (g n)").rearrange("p (h c) -> p h c", h=8); cwkb = qTsb
            cv, cvb = k.sb("cv", [128, 8, 16]); ci, cib = k.sb("ci", [128, 8, 16], U32); cj, cjb = k.sb("cj", [128, 8, 16], U32)
            cif, cifb = k.sb("cif", [128, 8, 16]); cjf, cjfb = k.sb("cjf", [128, 8, 16])
            i1, i1b = k.sb("i1", [128, 8, 16]); i2, i2b = k.sb("i2", [128, 8, 16])
            E = wk[:].rearrange("p g n -> p (g n)").rearrange("p (h k i) -> p h k i", h=8, k=16); Eb = wkb
            eid, eidb = k.sb("eid", [128, 128], I32)
            gw, gwb = k.sb("gw", [128, 8, 16]); gs, gsb = k.sb("gs", [128, 8]); act, actb = k.sb("actp", [128, 128])
            NG = 8
            gbuf = [k.sb(f"gath{i}", [128, 2 * D], BF16) for i in range(NG)]
            wact, wactb = k.sb("wact", [128, 128])
            junks = [(junk, junkb)] + [k.sb(f"junkp{i}", [128, D]) for i in range(3)]
            accs = [k.sb(f"accs{i}", [128, D]) for i in range(4)]
            acc, accb = k.sb("accp", [128, D])
            cur_kind = None; ng = 0
            for tt in range(NT):
                kind = "ctx" if tt < NLT else "lat"
                if kind == "ctx" and last:
                    continue
                if kind != cur_kind:
                    cur_kind = kind
                    r_ = NB if kind == "ctx" else b
                    k.dma("sp", lambda e, r_=r_: e.dma_start(out=A2[:], in_=self.mod_d[r_, 4 * D:5 * D].partition_broadcast(128)), [self.mod_b], [A2b])
                    k.op("dve", lambda e: e.scalar_tensor_tensor(out=A2[:], in0=A2[:], scalar=1.0, in1=g2[:], op0=ALU.add, op1=ALU.mult), [A2b, g2b], [A2b])
                    k.dma("sp", lambda e, r_=r_: e.dma_start(out=B2[:], in_=self.mod_d[r_, 3 * D:4 * D].partition_broadcast(128)), [self.mod_b], [B2b])
                    k.dma("sp", lambda e, r_=r_: e.dma_start(out=gt2[:], in_=self.mod_d[r_, 5 * D:6 * D].partition_broadcast(128)), [self.mod_b], [gt2b])
                t0 = tt * 128
                k.dma("sp", lambda e, t0=t0: e.dma_start(out=xt[:], in_=self.xs[b, t0:t0 + 128, :]), [self.xs_b], [xtb])
                k.op("act", lambda e: e.activation(out=junk[:], in_=xt[:], func=AF.Square, accum_out=ss[:]), [xtb], [junkb, ssb])
                self.rstd(ss[:], ssb, D)
                k.op("dve", lambda e: e.scalar_tensor_tensor(out=h2[:], in0=xt[:], scalar=ss[:, 0:1], in1=A2[:], op0=ALU.mult, op1=ALU.mult), [xtb, ssb, A2b], [h2b])
                k.op("pool", lambda e: e.tensor_tensor(out=h2[:], in0=h2[:], in1=B2[:], op=ALU.add), [h2b, B2b], [h2b])
                k.op("act", lambda e: e.activation(out=h16[:], in_=h2[:], func=AF.Copy), [h2b], [h16b])
                pt, ptb = self.psT[tt % 2]
                for kk in range(KD):
                    k.op("pe", lambda e, pt=pt, kk=kk: e.transpose(out=pt[:, kk, :], in_=h16[:, kk * 128:(kk + 1) * 128], identity=self.identb[:]), [h16b, self.identb_b], [ptb])
                self.evac(h2T[:], pt[:], [ptb], [h2Tb])
                for g in range(16):
                    ps, psb = self.psA[g // 4]
                    for kk in range(KD):
                        k.op("pe", lambda e, ps=ps, g=g, kk=kk: e.matmul(ps[:, (g % 4) * 128:(g % 4 + 1) * 128], lhsT=wq[:, kk, g * 128:(g + 1) * 128], rhs=h2T[:, kk, :], start=(kk == 0), stop=(kk == KD - 1)), [wqb, h2Tb], [psb])
                    if g % 4 == 3:
                        self.evac(qTs[:, g - 3:g + 1, :], ps[:].rearrange("p (g q) -> p g q", g=4), [psb], [qTsb])
                for g in range(16):
                    ps, psb = self.psA[g // 4]
                    k.op("pe", lambda e, ps=ps, g=g: e.matmul(ps[:, (g % 4) * 128:(g % 4 + 1) * 128], lhsT=qTs[:, g, :], rhs=keysT[:, g % 2, :], start=True, stop=True), [qTsb, keysTb], [psb])
                    if g % 4 == 3:
                        self.evac(sc[:, g - 3:g + 1, :], ps[:].rearrange("p (g q) -> p g q", g=4), [psb], [scb])
                m16bs = [Buf(f"m16_{g}") for g in range(16)]; wkbs = [Buf(f"wk_{g}") for g in range(16)]; ixbs = [Buf(f"ix_{g}") for g in range(16)]
                for g in range(16):
                    k.op("dve", lambda e, g=g: e.max(out=m16[:, g, 0:8], in_=sc[:, g, :]), [scb], [m16bs[g]])
                for g in range(16):
                    k.op("dve", lambda e, g=g: e.match_replace(out=wk[:, g, :], in_to_replace=m16[:, g, 0:8], in_values=sc[:, g, :], imm_value=-1e30), [scb, m16bs[g]], [wkbs[g], wkb])
                for g in range(16):
                    k.op("dve", lambda e, g=g: e.max(out=m16[:, g, 8:16], in_=wk[:, g, :]), [wkbs[g]], [m16bs[g]])
                for g in range(16):
                    k.op("dve", lambda e, g=g: e.max_index(out=ix[:, g, 0:8], in_max=m16[:, g, 0:8], in_values=sc[:, g, :]), [scb, m16bs[g]], [ixbs[g]])
                for g in range(16):
                    k.op("dve", lambda e, g=g: e.max_index(out=ix[:, g, 8:16], in_max=m16[:, g, 8:16], in_values=wk[:, g, :]), [wkbs[g], m16bs[g]], [ixbs[g], ixb, m16b])
                k.op("dve", lambda e: e.tensor_copy(out=ixf[:], in_=ix[:]), [ixb], [ixfb])
                m4 = m16[:].rearrange("p (h f) k -> p h f k", f=2)
                c4 = cand[:].rearrange("p h (i j) -> p h i j", i=16)
                k.op("dve", lambda e, m4=m4, c4=c4: e.tensor_tensor(out=c4, in0=m4[:, :, 0, :].unsqueeze(3).to_broadcast([128, 8, 16, 16]), in1=m4[:, :, 1, :].unsqueeze(2).to_broadcast([128, 8, 16, 16]), op=ALU.add), [m16b], [candb])
                cvbs = [Buf(f"cv_{h_}") for h_ in range(8)]; cwkbs = [Buf(f"cwk_{h_}") for h_ in range(8)]; cibs = [Buf(f"ci_{h_}") for h_ in range(8)]
                for hh in range(8):
                    k.op("dve", lambda e, hh=hh: e.max(out=cv[:, hh, 0:8], in_=cand[:, hh, :]), [candb], [cvbs[hh]])
                for hh in range(8):
                    k.op("dve", lambda e, hh=hh: e.match_replace(out=cwk[:, hh, :], in_to_replace=cv[:, hh, 0:8], in_values=cand[:, hh, :], imm_value=-1e30), [candb, cvbs[hh]], [cwkbs[hh], cwkb])
                for hh in range(8):
                    k.op("dve", lambda e, hh=hh: e.max(out=cv[:, hh, 8:16], in_=cwk[:, hh, :]), [cwkbs[hh]], [cvbs[hh]])
                for hh in range(8):
                    k.op("dve", lambda e, hh=hh: e.max_index(out=ci[:, hh, 0:8], in_max=cv[:, hh, 0:8], in_values=cand[:, hh, :]), [candb, cvbs[hh]], [cibs[hh]])
                for hh in range(8):
                    k.op("dve", lambda e, hh=hh: e.max_index(out=ci[:, hh, 8:16], in_max=cv[:, hh, 8:16], in_values=cwk[:, hh, :]), [cwkbs[hh], cvbs[hh]], [cibs[hh], cib, cvb])
                k.op("dve", lambda e: e.tensor_single_scalar(out=cj[:], in_=ci[:], scalar=15, op=ALU.bitwise_and), [cib], [cjb])
                k.op("dve", lambda e: e.tensor_single_scalar(out=ci[:], in_=ci[:], scalar=4, op=ALU.logical_shift_right), [cib], [cib])
                k.op("dve", lambda e: e.tensor_copy(out=cif[:], in_=ci[:]), [cib], [cifb])
                k.op("dve", lambda e: e.tensor_copy(out=cjf[:], in_=cj[:]), [cjb], [cjfb])
                x4 = ixf[:].rearrange("p (h f) k -> p h f k", f=2)
                for (cf, cfb, hf, dst, dstb) in ((cif, cifb, 0, i1, i1b), (cjf, cjfb, 1, i2, i2b)):
                    k.op("dve", lambda e, cf=cf: e.tensor_tensor(out=E, in0=cf[:].unsqueeze(3).to_broadcast([128, 8, 16, 16]), in1=iot[:].unsqueeze(1).unsqueeze(1).to_broadcast([128, 8, 16, 16]), op=ALU.is_equal), [cfb, iotb], [Eb])
                    k.op("dve", lambda e, hf=hf, x4=x4: e.tensor_tensor(out=E, in0=E, in1=x4[:, :, hf, :].unsqueeze(2).to_broadcast([128, 8, 16, 16]), op=ALU.mult), [Eb, ixfb], [Eb])
                    k.op("dve", lambda e, dst=dst: e.tensor_reduce(out=dst[:], in_=E, axis=AX.X, op=ALU.add), [Eb], [dstb])
                k.op("dve", lambda e: e.scalar_tensor_tensor(out=i1[:], in0=i1[:], scalar=128.0, in1=i2[:], op0=ALU.mult, op1=ALU.add), [i1b, i2b], [i1b])
                k.op("dve", lambda e: e.tensor_scalar(out=eid[:].rearrange("p (h k) -> p h k", h=8), in0=i1[:], scalar1=0.0, scalar2=None, op0=ALU.add), [i1b], [eidb])
                k.op("dve", lambda e: e.tensor_tensor(out=gw[:], in0=cv[:], in1=cv[:, :, 0:1].to_broadcast([128, 8, 16]), op=ALU.subtract), [cvb], [gwb])
                k.op("act", lambda e: e.activation(out=gw[:], in_=gw[:], func=AF.Exp), [gwb], [gwb])
                k.op("dve", lambda e: e.tensor_reduce(out=gs[:], in_=gw[:], axis=AX.X, op=ALU.add), [gwb], [gsb])
                k.op("dve", lambda e: e.reciprocal(out=gs[:], in_=gs[:]), [gsb], [gsb])
                k.op("dve", lambda e: e.tensor_tensor(out=gw[:], in0=gw[:], in1=gs[:].unsqueeze(2).to_broadcast([128, 8, 16]), op=ALU.mult), [gwb, gsb], [gwb])
                G = 4
                actbs = [Buf(f"act_{i_}") for i_ in range(128)]
                for s0 in range(0, 128, G):
                    grp = []
                    for sl_ in range(s0, s0 + G):
                        gb_, gbb_ = gbuf[ng % NG]; ng += 1
                        grp.append((sl_, gb_, gbb_))
                        jk_, jkb_ = junks[sl_ % 4]
                        k.dma("pool", lambda e, gb_=gb_, sl_=sl_: e.indirect_dma_start(out=gb_[:, :], out_offset=None, in_=self.uvb[:, :], in_offset=bass.IndirectOffsetOnAxis(ap=eid[:, sl_:sl_ + 1], axis=0)), [eidb, self.uvb_b], [gbb_])
                        k.op("dve", lambda e, gb_=gb_, sl_=sl_, jk_=jk_: e.scalar_tensor_tensor(out=jk_[:], in0=gb_[:, 0:D], scalar=1.0, in1=h2[:], op0=ALU.mult, op1=ALU.mult, accum_out=act[:, sl_:sl_ + 1]), [gbb_, h2b], [jkb_, actbs[sl_]])
                    k.op("act", lambda e, s0=s0: e.activation(out=wact[:, s0:s0 + G], in_=act[:, s0:s0 + G], func=AF.Gelu), actbs[s0:s0 + G], [wactb])
                    k.op("pool", lambda e, s0=s0: e.tensor_tensor(out=wact[:, s0:s0 + G], in0=wact[:, s0:s0 + G], in1=gw[:].rearrange("p h k -> p (h k)")[:, s0:s0 + G], op=ALU.mult), [wactb, gwb], [wactb])
                    for (sl_, gb_, gbb_) in grp:
                        ac_, acb_ = accs[sl_ % 4]
                        if sl_ < 4:
                            k.op("dve", lambda e, gb_=gb_, sl_=sl_, ac_=ac_: e.tensor_scalar(out=ac_[:], in0=gb_[:, D:2 * D], scalar1=wact[:, sl_:sl_ + 1], scalar2=None, op0=ALU.mult), [gbb_, wactb], [acb_])
                        else:
                            k.op("dve", lambda e, gb_=gb_, sl_=sl_, ac_=ac_: e.scalar_tensor_tensor(out=ac_[:], in0=gb_[:, D:2 * D], scalar=wact[:, sl_:sl_ + 1], in1=ac_[:], op0=ALU.mult, op1=ALU.add), [gbb_, wactb, acb_], [acb_])
                k.op("pool", lambda e: e.tensor_tensor(out=accs[0][0][:], in0=accs[0][0][:], in1=accs[1][0][:], op=ALU.add), [accs[0][1], accs[1][1]], [accs[0][1]])
                k.op("dve", lambda e: e.tensor_tensor(out=accs[2][0][:], in0=accs[2][0][:], in1=accs[3][0][:], op=ALU.add), [accs[2][1], accs[3][1]], [accs[2][1]])
                k.op("pool", lambda e: e.tensor_tensor(out=acc[:], in0=accs[0][0][:], in1=accs[2][0][:], op=ALU.add), [accs[0][1], accs[2][1]], [accb])
                k.op("pool", lambda e: e.tensor_tensor(out=acc[:], in0=acc[:], in1=gt2[:], op=ALU.mult), [accb, gt2b], [accb])
                k.op("pool", lambda e: e.tensor_tensor(out=xt[:], in0=xt[:], in1=acc[:], op=ALU.add), [xtb, accb], [xtb])
                k.dma("sp", lambda e, t0=t0: e.dma_start(out=self.xs[b, t0:t0 + 128, :], in_=xt[:]), [xtb], [self.xs_b])
        k.es = k.es0
        k.barrier()


def _rope_tables(S):
    rows = S // 64
    row = np.repeat(np.arange(rows), 64).astype(np.float32)
    col = np.tile(np.arange(64), rows).astype(np.float32)
    inv = np.power(np.float32(10000.0), -np.arange(16, dtype=np.float32) / 16).astype(np.float32)
    ar = row[:, None] * inv
    ac = col[:, None] * inv
    ang = np.concatenate([ar, ar, ac, ac], axis=-1).astype(np.float32)
    cos = np.cos(ang).astype(np.float32)
    sin = np.sin(ang).astype(np.float32).reshape(-1, 2, 2, 16).copy()
    sin[:, :, 0, :] *= -1.0
    return cos, sin.reshape(-1, 64)


def _const_masks():
    p = np.arange(128)[:, None]; f = np.arange(128)[None, :]
    same = (p // 64) == (f // 64)
    m = np.zeros((8, 128, 128), np.float32)
    m[0] = same & (p <= f); m[1] = same & (p >= f); m[2] = same & (p < f); m[3] = same & (p > f)
    m[4] = same; m[5] = (p < 64) & (f >= 0); m[6] = (p >= 64) & (f >= 0)
    return (p >= f).astype(np.float32), (p <= f).astype(np.float32), m


def kernel(**inputs):
    from concourse.bass_utils import run_bass_kernel_spmd
    x = np.asarray(inputs["x"], dtype=np.float32)
    B, S, _ = x.shape
    L = inputs["ctx"].shape[1]
    DEPTH = inputs["w_in"].shape[0]
    m = M(dict(NB=1, S=S, L=L, DEPTH=DEPTH))
    nc = m.build()
    shared = {k_: np.ascontiguousarray(np.asarray(v, dtype=np.float32)) for k_, v in inputs.items()
              if k_ not in ("x", "c", "ctx")}
    shared["peer_uv"] = np.ascontiguousarray(np.concatenate(
        [shared.pop("peer_u").reshape(-1, D), shared.pop("peer_v").reshape(-1, D)], axis=1))
    shared["ident"] = np.eye(128, dtype=np.float32)
    shared["rope_cos"], shared["rope_sin"] = _rope_tables(S)
    shared["mask_lo"], shared["mask_hi"], shared["gmasks"] = _const_masks()
    c = np.asarray(inputs["c"], dtype=np.float32)
    ctx = np.asarray(inputs["ctx"], dtype=np.float32)
    in_maps = []
    for b in range(B):
        im = dict(shared)
        im["x"] = np.ascontiguousarray(x[b:b + 1])
        im["c"] = np.ascontiguousarray(c[b:b + 1])
        im["ctx"] = np.ascontiguousarray(ctx[b:b + 1])
        in_maps.append(im)
    res = run_bass_kernel_spmd(nc, in_maps, core_ids=list(range(B)))
    return np.concatenate([np.asarray(r["out"], dtype=np.float32) for r in res.results], axis=0)
```

```python
import numpy as np
from contextlib import ExitStack
import concourse.bass as bass
import concourse.mybir as mybir

F32 = mybir.dt.float32
BF16 = mybir.dt.bfloat16
I32 = mybir.dt.int32
U32 = mybir.dt.uint32
AF = mybir.ActivationFunctionType
ALU = mybir.AluOpType
AX = mybir.AxisListType


class Buf:
    __slots__ = ("name", "w", "r", "excl")

    def __init__(self, name, excl=False):
        self.name = name
        self.w = None
        self.r = {}
        self.excl = excl


class KB:
    ENG = ("pe", "dve", "act", "pool", "sp")

    def __init__(self, nc, es):
        self.nc = nc
        self.es = es
        self.eng = {"pe": nc.tensor, "dve": nc.vector, "act": nc.scalar, "pool": nc.gpsimd, "sp": nc.sync}
        self.ops = {e: [] for e in self.ENG}
        self.cnt = {e: 0 for e in self.ENG}
        self.seen = {e: {} for e in self.ENG}
        self.csem = {e: es.enter_context(nc.semaphore("c_" + e)) for e in self.ENG}
        self.NS = 16
        self.DQ = ("sp", "pool")
        self.dsem = {e: [es.enter_context(nc.semaphore(f"d_{e}{j}")) for j in range(self.NS)] for e in self.DQ}
        self.dcnt = {e: [0] * self.NS for e in self.DQ}
        self.dtot = {e: 0 for e in self.DQ}
        self.nbuf = 0

    def sb(self, name, shape, dt=F32):
        self.nbuf += 1
        name = f"s{self.nbuf}_{name}"
        t = self.es.enter_context(self.nc.sbuf_tensor(name, list(shape), dt))
        return t, Buf(name)

    def ps(self, name, shape, dt=F32):
        self.nbuf += 1
        name = f"p{self.nbuf}_{name}"
        t = self.es.enter_context(self.nc.psum_tensor(name, list(shape), dt))
        return t, Buf(name, excl=True)

    def dram(self, name, shape, dt=F32, kind="Internal"):
        t = self.nc.dram_tensor(name, list(shape), dt, kind=kind)
        return t.ap(), Buf(name)

    def _waits(self, E, reads, writes):
        toks = []
        for b in reads:
            if b.w is not None:
                toks.append(b.w)
        for b in writes:
            if b.w is not None:
                toks.append(b.w)
            toks.extend(b.r.values())
        waits = {}
        for kind, E2, idx in toks:
            if kind == "c":
                if E2 == E and E == "pe":
                    continue
                sem, val, key = self.csem[E2], idx, "c_" + E2
            else:
                j, m = idx
                sem, val, key = self.dsem[E2][j], 16 * m, f"d_{E2}{j}"
            if self.seen[E].get(key, 0) >= val:
                continue
            if key not in waits or waits[key][1] < val:
                waits[key] = (sem, val)
        for key, (sem, val) in waits.items():
            self.seen[E][key] = val
        return list(waits.values())

    def op(self, E, fn, reads=(), writes=()):
        ex = [b for b in reads if b.excl]
        if ex:
            reads = [b for b in reads if not b.excl]
            writes = list(writes) + ex
        waits = self._waits(E, reads, writes)
        self.cnt[E] += 1
        idx = self.cnt[E]
        self.ops[E].append((waits, fn, self.csem[E], 1))
        tok = ("c", E, idx)
        for b in reads:
            b.r[("c", E)] = tok
        for b in writes:
            b.w = tok
            b.r = {}

    def dma(self, Q, fn, reads=(), writes=()):
        waits = self._waits(Q, reads, writes)
        j = self.dtot[Q] % self.NS
        self.dtot[Q] += 1
        key = f"d_{Q}{j}"
        if self.dcnt[Q][j] > 0 and self.seen[Q].get(key, 0) < 16 * self.dcnt[Q][j]:
            waits.append((self.dsem[Q][j], 16 * self.dcnt[Q][j]))
            self.seen[Q][key] = 16 * self.dcnt[Q][j]
        self.dcnt[Q][j] += 1
        self.ops[Q].append((waits, fn, self.dsem[Q][j], 16))
        tok = ("d", Q, (j, self.dcnt[Q][j]))
        for b in reads:
            b.r[("d", Q, j)] = tok
        for b in writes:
            b.w = tok
            b.r = {}

    def barrier(self):
        for E in self.ENG:
            waits = []
            for E2 in self.ENG:
                if E2 != E and self.cnt[E2] > self.seen[E].get("c_" + E2, 0):
                    waits.append((self.csem[E2], self.cnt[E2]))
                    self.seen[E]["c_" + E2] = self.cnt[E2]
                if E2 in self.DQ:
                    for j in range(self.NS):
                        key = f"d_{E2}{j}"
                        if self.dcnt[E2][j] * 16 > self.seen[E].get(key, 0):
                            waits.append((self.dsem[E2][j], 16 * self.dcnt[E2][j]))
                            self.seen[E][key] = 16 * self.dcnt[E2][j]
            if E != "pe" and self.cnt[E] > self.seen[E].get("c_" + E, 0):
                waits.append((self.csem[E], self.cnt[E]))
                self.seen[E]["c_" + E] = self.cnt[E]
            if waits:
                self.ops[E].append((waits, None, None, 0))

    def emit(self):
        self.barrier()
        nc = self.nc
        with nc.Block() as block:
            def mk(E):
                def body(eng):
                    for waits, fn, sem, inc in self.ops[E]:
                        for s, v in waits:
                            eng.wait_ge(s, v)
                        if fn is not None:
                            fn(eng).then_inc(sem, inc)
                return body
            block.tensor(mk("pe"))
            block.vector(mk("dve"))
            block.scalar(mk("act"))
            block.gpsimd(mk("pool"))
            block.sync(mk("sp"))

    def ninstr(self):
        return {e: len(self.ops[e]) for e in self.ENG}


import math
import numpy as np
from contextlib import ExitStack

D = 1024
KD = 8
IN_COLS = 7440
C_QA, C_KA, C_VA, C_QB, C_KB, C_VB, C_QKVC, C_ZC, C_AC, C_BC, C_GL = 0, 512, 1024, 1536, 2048, 2176, 2304, 3840, 4352, 4360, 4368
EPS = 1e-6


class StopPhase(Exception):
    pass


class M:
    def __init__(self, cfg):
        self.cfg = cfg
        self.NB = cfg["NB"]; self.S = cfg["S"]; self.L = cfg["L"]; self.DEPTH = cfg["DEPTH"]
        self.T = self.S + self.L
        self.NT = self.T // 128
        self.NLT = self.L // 128
        self.stop_after = cfg.get("stop_after", "all")

    def decl_inputs(self, nc):
        NB, S, L, DEPTH = self.NB, self.S, self.L, self.DEPTH
        shapes = dict(
            x=[NB, S, D], c=[NB, D], ctx=[NB, L, D], c_ctx=[D], norm1_g=[DEPTH, D], norm2_g=[DEPTH, D],
            w_ada=[DEPTH, D, 6 * D], b_ada=[DEPTH, 6 * D], w_in=[DEPTH, D, IN_COLS], b_gate=[DEPTH, 3 * D],
            qk_norm_g=[DEPTH, 4, 64], diff_lam=[DEPTH, 2, 2, 64], diff_subln_g=[DEPTH, 128], wg_sink=[DEPTH, 8],
            gd_conv_w=[DEPTH, 5, 1536], gd_a_log=[DEPTH, 2, 4], gd_dt_bias=[DEPTH, 2, 4], gd_norm_g=[DEPTH, 128],
            w_branch=[DEPTH, 3, 512, D], w_out=[DEPTH, D, D], peer_wq=[DEPTH, D, 2048], peer_keys=[DEPTH, 2, 128, 128],
            peer_uv=[DEPTH * 16384, 2 * D],
            ident=[128, 128], rope_cos=[S, 64], rope_sin=[S, 64], mask_lo=[128, 128], mask_hi=[128, 128], gmasks=[8, 128, 128],
        )
        self.inp = {n: nc.dram_tensor(n, s, F32, kind="ExternalInput").ap() for n, s in shapes.items()}
        self.out = nc.dram_tensor("out", [NB, S, D], F32, kind="ExternalOutput").ap()

    def build(self):
        nc = bass.Bass("TRN2", target_bir_lowering=False)
        self.nc = nc
        self.decl_inputs(nc)
        with ExitStack() as es:
            k = KB(nc, es)
            self.k = k
            k.es0 = es
            self.alloc()
            self.prologue()
            for l in range(self.DEPTH):
                self.layer(l)
            self.epilogue()
            k.emit()
            print("instr counts", k.ninstr())
        return nc

    def alloc(self):
        k = self.k; NB, T = self.NB, self.T
        dk = "ExternalOutput" if self.cfg.get("debug") else "Internal"
        self.xs, self.xs_b = k.dram("xs", [NB, T, D], kind=dk)
        self.mod_d, self.mod_b = k.dram("mod_d", [NB + 1, 6 * D], kind=dk)
        self.proj_d, self.proj_b = k.dram("proj_d", [1, T, IN_COLS], kind=dk)
        self.qkvT_d, self.qkvT_b = k.dram("qkvT_d", [1, 1536, T], kind=dk)
        self.oT_d, self.oT_b = k.dram("oT_d", [1, 1536, T], BF16, kind=dk)
        self.uvb, self.uvb_b = k.dram("uvb", [16384, 2 * D], BF16)
        self.ident, self.ident_b = k.sb("ident", [128, 128])
        self.identb, self.identb_b = k.sb("identb", [128, 128], BF16)
        self.ev = 0

    def psum_std(self):
        k = self.k
        self.psA = [k.ps(f"psA{i}", [128, 512]) for i in range(6)]
        self.psT = [k.ps(f"psT{i}", [128, 8, 128], BF16) for i in range(2)]

    def evac(self, out_ap, in_ap, reads, writes, func=None):
        k = self.k
        self.ev ^= 1
        if self.ev:
            k.op("act", lambda e: e.activation(out=out_ap, in_=in_ap, func=AF.Copy), reads, writes)
        else:
            k.op("dve", lambda e: e.tensor_copy(out=out_ap, in_=in_ap), reads, writes)

    def prologue(self):
        k = self.k; inp = self.inp; NB, L, S = self.NB, self.L, self.S
        k.dma("sp", lambda e: e.dma_start(out=self.ident[:], in_=inp["ident"][:, :]), [], [self.ident_b])
        k.op("dve", lambda e: e.tensor_copy(out=self.identb[:], in_=self.ident[:]), [self.ident_b], [self.identb_b])
        for b in range(NB):
            k.dma("sp", lambda e, b=b: e.dma_start(out=self.xs[b, 0:L, :], in_=inp["ctx"][b, :, :]), [], [self.xs_b])
            k.dma("sp", lambda e, b=b: e.dma_start(out=self.xs[b, L:L + S, :], in_=inp["x"][b, :, :]), [], [self.xs_b])

    def epilogue(self):
        k = self.k; NB, L, S = self.NB, self.L, self.S
        for b in range(NB):
            k.dma("sp", lambda e, b=b: e.dma_start(out=self.out[b, :, :], in_=self.xs[b, L:L + S, :]), [self.xs_b], [])

    def phase_T(self, l):
        k = self.k; inp = self.inp
        k.barrier()
        with ExitStack() as es:
            k.es = es
            RB = 2
            tf = [k.sb(f"tf{i}", [128, RB, 2 * D]) for i in range(3)]
            tb = [k.sb(f"tb{i}", [128, RB, 2 * D], BF16) for i in range(3)]
            engs = ("dve", "act", "pool")
            for i, r0 in enumerate(range(0, 16384, 128 * RB)):
                f_, fb_ = tf[i % 3]; b_, bb_ = tb[i % 3]
                src = inp["peer_uv"][l * 16384 + r0:l * 16384 + r0 + 128 * RB, :].rearrange("(p j) c -> p j c", j=RB)
                dst = self.uvb[r0:r0 + 128 * RB, :].rearrange("(p j) c -> p j c", j=RB)
                k.dma("sp", lambda e, f_=f_, src=src: e.dma_start(out=f_[:], in_=src), [], [fb_])
                E_ = engs[i % 3]
                if E_ == "act":
                    k.op("act", lambda e, f_=f_, b_=b_: e.activation(out=b_[:], in_=f_[:], func=AF.Copy), [fb_], [bb_])
                else:
                    k.op(E_, lambda e, f_=f_, b_=b_: e.tensor_copy(out=b_[:], in_=f_[:]), [fb_], [bb_])
                k.dma("sp", lambda e, b_=b_, dst=dst: e.dma_start(out=dst, in_=b_[:]), [bb_], [self.uvb_b])
        k.es = k.es0
        k.barrier()

    def layer(self, l):
        self.phase_ada(l)
        if self.stop_after not in ("ada", "proj", "A", "B", "C", "M"):
            self.phase_T(l)
        if self.stop_after == "ada":
            return
        for b in range(self.NB):
            self.phase_proj(l, b)
            if self.stop_after == "proj":
                continue
            self.phase_A(l, b)
            if self.stop_after == "A":
                continue
            self.phase_B(l, b)
            if self.stop_after == "B":
                continue
            self.phase_C(l, b)
            if self.stop_after == "C":
                continue
            self.phase_M(l, b)
            if self.stop_after == "M":
                continue
            self.phase_P(l, b)

    def phase_ada(self, l):
        k = self.k; inp = self.inp; NB = self.NB; R = NB + 1
        k.barrier()
        with ExitStack() as es:
            k.es = es
            self.psum_std()
            cT, cTb = k.sb("cT", [128, KD, R])
            wt = [k.sb(f"adaw{i}", [128, KD, 512]) for i in range(2)]
            bt = [k.sb(f"adab{i}", [R, 512]) for i in range(2)]
            ot = [k.sb(f"adao{i}", [R, 512]) for i in range(2)]
            for n_ in range(NB):
                k.dma("sp", lambda e, n_=n_: e.dma_start(out=cT[:, :, n_:n_ + 1], in_=inp["c"][n_, :].rearrange("(k p o) -> p k o", p=128, o=1), allow_slow_non_contiguous=True), [], [cTb])
            k.dma("sp", lambda e: e.dma_start(out=cT[:, :, NB:R], in_=inp["c_ctx"].rearrange("(k p o) -> p k o", p=128, o=1), allow_slow_non_contiguous=True), [], [cTb])
            k.op("act", lambda e: e.activation(out=cT[:], in_=cT[:], func=AF.Silu), [cTb], [cTb])
            for cb in range(12):
                w, wb = wt[cb % 2]; bb, bbb = bt[cb % 2]; oo, oob = ot[cb % 2]
                ps, psb = self.psA[cb % 2]
                c0 = cb * 512
                k.dma("sp", lambda e, w=w, c0=c0: e.dma_start(out=w[:], in_=inp["w_ada"][l, :, c0:c0 + 512].rearrange("(k p) c -> p k c", p=128)), [], [wb])
                k.dma("pool", lambda e, bb=bb, c0=c0: e.dma_start(out=bb[:], in_=inp["b_ada"][l, c0:c0 + 512].partition_broadcast(R)), [], [bbb])
                for kk in range(KD):
                    k.op("pe", lambda e, kk=kk, w=w, ps=ps: e.matmul(ps[0:R, :], lhsT=cT[:, kk, :], rhs=w[:, kk, :], start=(kk == 0), stop=(kk == KD - 1)), [cTb, wb], [psb])
                k.op("dve", lambda e, oo=oo, ps=ps, bb=bb: e.tensor_tensor(out=oo[:], in0=ps[0:R, :], in1=bb[:], op=ALU.add), [psb, bbb], [oob])
                k.dma("sp", lambda e, oo=oo, c0=c0: e.dma_start(out=self.mod_d[:, c0:c0 + 512], in_=oo[:]), [oob], [self.mod_b])
        k.es = k.es0
        k.barrier()

    def mod_tile(self, es, name, l, r, j, plus_one=False, gname=None):
        k = self.k; inp = self.inp
        t, tb = k.sb(name, [128, D])
        k.dma("sp", lambda e: e.dma_start(out=t[:], in_=self.mod_d[r, j * D:(j + 1) * D].partition_broadcast(128)), [self.mod_b], [tb])
        if plus_one:
            g, gb = k.sb(name + "_g", [128, D])
            k.dma("pool", lambda e: e.dma_start(out=g[:], in_=inp[gname][l, :].partition_broadcast(128)), [], [gb])
            k.op("dve", lambda e: e.scalar_tensor_tensor(out=t[:], in0=t[:], scalar=1.0, in1=g[:], op0=ALU.add, op1=ALU.mult), [tb, gb], [tb])
        return t, tb

    def rstd(self, ss, ssb, n, reads=()):
        k = self.k
        k.op("act", lambda e: e.activation(out=ss, in_=ss, func=AF.Ln, scale=1.0 / n, bias=EPS), [ssb], [ssb])
        k.op("act", lambda e: e.activation(out=ss, in_=ss, func=AF.Exp, scale=-0.5), [ssb], [ssb])

    def phase_proj(self, l, b):
        k = self.k; inp = self.inp; NB, T, NT, NLT = self.NB, self.T, self.NT, self.NLT
        k.barrier()
        with ExitStack() as es:
            k.es = es
            self.psum_std()
            hT, hTb = k.sb("hT", [128, KD, T], BF16)
            A1 = {}; B1 = {}
            for r, nm in ((b, "lat"), (NB, "ctx")):
                A1[nm] = self.mod_tile(es, "A1" + nm, l, r, 1, True, "norm1_g")
                B1[nm] = self.mod_tile(es, "B1" + nm, l, r, 0)
            xt = [k.sb(f"xt{i}", [128, D]) for i in range(2)]
            junk, junkb = k.sb("junk", [128, D])
            hh = [k.sb(f"hh{i}", [128, D], BF16) for i in range(2)]
            sst = [k.sb(f"ss{i}", [128, 1]) for i in range(2)]
            for tt in range(NT):
                nm = "ctx" if tt < NLT else "lat"
                x_, xb_ = xt[tt % 2]; h_, hb_ = hh[tt % 2]; s_, sb_ = sst[tt % 2]
                pt, ptb = self.psT[tt % 2]
                t0 = tt * 128
                k.dma("sp", lambda e, x_=x_, t0=t0: e.dma_start(out=x_[:], in_=self.xs[b, t0:t0 + 128, :]), [self.xs_b], [xb_])
                k.op("act", lambda e, x_=x_, s_=s_: e.activation(out=junk[:], in_=x_[:], func=AF.Square, accum_out=s_[:]), [xb_], [junkb, sb_])
                self.rstd(s_[:], sb_, D)
                a_, ab_ = A1[nm]; b_, bb_ = B1[nm]
                k.op("dve", lambda e, x_=x_, s_=s_, a_=a_: e.scalar_tensor_tensor(out=x_[:], in0=x_[:], scalar=s_[:, 0:1], in1=a_[:], op0=ALU.mult, op1=ALU.mult), [xb_, sb_, ab_], [xb_])
                k.op("pool", lambda e, x_=x_, h_=h_, b_=b_: e.tensor_tensor(out=h_[:], in0=x_[:], in1=b_[:], op=ALU.add), [xb_, bb_], [hb_])
                for kk in range(KD):
                    k.op("pe", lambda e, kk=kk, h_=h_, pt=pt: e.transpose(out=pt[:, kk, :], in_=h_[:, kk * 128:(kk + 1) * 128], identity=self.identb[:]), [hb_, self.identb_b], [ptb])
                self.evac(hT[:, :, t0:t0 + 128], pt[:], [ptb], [hTb])
            wf = [k.sb(f"wf{i}", [128, KD, 512]) for i in range(2)]
            wb16 = [k.sb(f"wb{i}", [128, KD, 512], BF16) for i in range(2)]
            st = [k.sb(f"st{i}", [128, 512]) for i in range(3)]
            blocks = []
            for (a, z) in ((0, C_QKVC), (C_ZC, IN_COLS)):
                c0 = a
                while c0 < z:
                    cw = min(512, z - c0); blocks.append((c0, cw)); c0 += cw
            n = 0
            for bi, (c0, cw) in enumerate(blocks):
                w, wb_ = wf[bi % 2]; w16, w16b = wb16[bi % 2]
                k.dma("sp", lambda e, w=w, c0=c0, cw=cw: e.dma_start(out=w[:, :, 0:cw], in_=inp["w_in"][l, :, c0:c0 + cw].rearrange("(k p) c -> p k c", p=128)), [], [wb_])
                k.op("pool", lambda e, w=w, w16=w16, cw=cw: e.tensor_copy(out=w16[:, :, 0:cw], in_=w[:, :, 0:cw]), [wb_], [w16b])
                for tt in range(NT):
                    ps, psb = self.psA[n % 4]; s_, sb_ = st[n % 3]; n += 1
                    t0 = tt * 128
                    for kk in range(KD):
                        k.op("pe", lambda e, kk=kk, ps=ps, w16=w16, t0=t0, cw=cw: e.matmul(ps[:, 0:cw], lhsT=hT[:, kk, t0:t0 + 128], rhs=w16[:, kk, 0:cw], start=(kk == 0), stop=(kk == KD - 1)), [hTb, w16b], [psb])
                    self.evac(s_[:, 0:cw], ps[:, 0:cw], [psb], [sb_])
                    k.dma("sp", lambda e, s_=s_, t0=t0, c0=c0, cw=cw: e.dma_start(out=self.proj_d[0, t0:t0 + 128, c0:c0 + cw], in_=s_[:, 0:cw]), [sb_], [self.proj_b])
            TB = 512
            for chb in range(12):
                w, wb_ = wf[chb % 2]; w16, w16b = wb16[chb % 2]
                c0 = C_QKVC + chb * 128
                k.dma("sp", lambda e, w=w, c0=c0: e.dma_start(out=w[:, :, 0:128], in_=inp["w_in"][l, :, c0:c0 + 128].rearrange("(k p) c -> p k c", p=128)), [], [wb_])
                k.op("pool", lambda e, w=w, w16=w16: e.tensor_copy(out=w16[:, :, 0:128], in_=w[:, :, 0:128]), [wb_], [w16b])
                for t0 in range(0, T, TB):
                    tn = min(TB, T - t0)
                    ps, psb = self.psA[n % 4]; s_, sb_ = st[n % 3]; n += 1
                    for kk in range(KD):
                        k.op("pe", lambda e, kk=kk, ps=ps, w16=w16, t0=t0, tn=tn: e.matmul(ps[:, 0:tn], lhsT=w16[:, kk, 0:128], rhs=hT[:, kk, t0:t0 + tn], start=(kk == 0), stop=(kk == KD - 1)), [hTb, w16b], [psb])
                    self.evac(s_[:, 0:tn], ps[:, 0:tn], [psb], [sb_])
                    k.dma("sp", lambda e, s_=s_, t0=t0, chb=chb, tn=tn: e.dma_start(out=self.qkvT_d[0, chb * 128:(chb + 1) * 128, t0:t0 + tn], in_=s_[:, 0:tn]), [sb_], [self.qkvT_b])
        k.es = k.es0
        k.barrier()

    def phase_A(self, l, b):
        k = self.k; inp = self.inp; NB, T, NT, NLT, L, S = self.NB, self.T, self.NT, self.NLT, self.L, self.S
        lam_init = 0.8 - 0.6 * math.exp(-0.3 * l)
        k.barrier()
        with ExitStack() as es:
            k.es = es
            self.psum_std()
            qT, qTb = k.sb("qT", [128, 4, T], BF16)
            kT, kTb = k.sb("kT", [128, 4, T], BF16)
            vA, vAb = k.sb("vA", [128, NT, 512], BF16)
            onesb, onesbb = k.sb("onesb", [128, 128], BF16)
            onesf, onesfb = k.sb("onesf", [128, 128])
            k.op("pool", lambda e: e.memset(onesb[:], 1.0), [], [onesbb])
            k.op("pool", lambda e: e.memset(onesf[:], 1.0), [], [onesfb])
            lv, lvb = k.sb("lv", [128, 256]); lp, lpb = k.sb("lp", [128, 128]); le, leb = k.sb("le", [128, 2]); nlam, nlamb = k.sb("nlam", [128, 1])
            k.dma("sp", lambda e: e.dma_start(out=lv[:], in_=inp["diff_lam"][l].rearrange("a b c -> (a b c)").partition_broadcast(128)), [], [lvb])
            lv4 = lv[:].rearrange("p (m j c) -> p m j c", m=2, j=2)
            k.op("dve", lambda e: e.tensor_tensor(out=lp[:].rearrange("p (m c) -> p m c", m=2), in0=lv4[:, :, 0, :], in1=lv4[:, :, 1, :], op=ALU.mult), [lvb], [lpb])
            k.op("dve", lambda e: e.tensor_reduce(out=le[:], in_=lp[:].rearrange("p (m c) -> p m c", m=2), axis=AX.X, op=ALU.add), [lpb], [leb])
            k.op("act", lambda e: e.activation(out=le[:], in_=le[:], func=AF.Exp), [leb], [leb])
            k.op("dve", lambda e: e.scalar_tensor_tensor(out=nlam[:], in0=le[:, 1:2], scalar=-lam_init, in1=le[:, 0:1], op0=ALU.add, op1=ALU.subtract), [leb], [nlamb])
            gqk, gqkb = k.sb("gqk", [128, 16, 64])
            for g in range(16):
                k.dma("pool", lambda e, g=g: e.dma_start(out=gqk[:, g, :], in_=inp["qk_norm_g"][l, 0 if g < 8 else 1, :].partition_broadcast(128)), [], [gqkb])
            sg, sgb = k.sb("sg", [128, 1])
            k.dma("sp", lambda e: e.dma_start(out=sg[:], in_=inp["diff_subln_g"][l, :].rearrange("(p o) -> p o", o=1)), [], [sgb])
            self.qk_prep(l, b, C_QA, 16, gqk, gqkb, [(qT, qTb, 0, 4), (kT, kTb, 8, 4)])
            vf = [k.sb(f"vf{i}", [128, 512]) for i in range(2)]
            for tt in range(NT):
                v_, vb_ = vf[tt % 2]
                k.dma("sp", lambda e, v_=v_, tt=tt: e.dma_start(out=v_[:], in_=self.proj_d[0, tt * 128:(tt + 1) * 128, C_VA:C_VA + 512]), [self.proj_b], [vb_])
                k.op("pool", lambda e, v_=v_, tt=tt: e.tensor_copy(out=vA[:, tt, :], in_=v_[:]), [vb_], [vAb])
            pT = [k.sb(f"pT{i}", [128, 512], BF16) for i in range(3)]
            rs = [k.sb(f"rs{i}", [128, 512]) for i in range(2)]
            o1, o1b = k.sb("o1", [128, 512]); o2, o2b = k.sb("o2", [128, 512]); sq, sqb = k.sb("sq", [128, 512])
            ob = [k.sb(f"ob{i}", [128, 512], BF16) for i in range(2)]
            qblocks = [(0, L, 0, NLT)] + [(q0, min(512, T - q0), 0, NT) for q0 in range(L, T, 512)]
            n = 0; nob = 0
            for h in range(4):
                for (q0, qn, kt0, kt1) in qblocks:
                    psO = [self.psA[2], self.psA[3]]; psS = [self.psA[4], self.psA[5]]
                    for kt in range(kt0, kt1):
                        for m in range(2):
                            ps, psb = self.psA[n % 2]; p_, pb_ = pT[n % 3]; n += 1
                            k.op("pe", lambda e, ps=ps, m=m, kt=kt, h=h, q0=q0, qn=qn: e.matmul(ps[:, 0:qn], lhsT=kT[64 * m:64 * m + 64, h, kt * 128:(kt + 1) * 128], rhs=qT[64 * m:64 * m + 64, h, q0:q0 + qn], start=True, stop=True), [kTb, qTb], [psb])
                            k.op("act", lambda e, ps=ps, p_=p_, qn=qn: e.activation(out=p_[:, 0:qn], in_=ps[:, 0:qn], func=AF.Exp, scale=0.125), [psb], [pb_])
                            k.op("pe", lambda e, m=m, kt=kt, h=h, p_=p_, qn=qn, kt0=kt0, kt1=kt1: e.matmul(psO[m][0][:, 0:qn], lhsT=vA[:, kt, h * 128:(h + 1) * 128], rhs=p_[:, 0:qn], start=(kt == kt0), stop=(kt == kt1 - 1)), [vAb, pb_], [psO[m][1]])
                            k.op("pe", lambda e, m=m, kt=kt, p_=p_, qn=qn, kt0=kt0, kt1=kt1: e.matmul(psS[m][0][:, 0:qn], lhsT=onesb[:], rhs=p_[:, 0:qn], start=(kt == kt0), stop=(kt == kt1 - 1)), [onesbb, pb_], [psS[m][1]])
                    for m in range(2):
                        k.op("dve", lambda e, m=m, qn=qn: e.reciprocal(out=rs[m][0][:, 0:qn], in_=psS[m][0][:, 0:qn]), [psS[m][1]], [rs[m][1]])
                    k.op("dve", lambda e, qn=qn: e.tensor_tensor(out=o1[:, 0:qn], in0=psO[0][0][:, 0:qn], in1=rs[0][0][:, 0:qn], op=ALU.mult), [psO[0][1], rs[0][1]], [o1b])
                    k.op("dve", lambda e, qn=qn: e.tensor_tensor(out=o2[:, 0:qn], in0=psO[1][0][:, 0:qn], in1=rs[1][0][:, 0:qn], op=ALU.mult), [psO[1][1], rs[1][1]], [o2b])
                    k.op("dve", lambda e, qn=qn: e.scalar_tensor_tensor(out=o1[:, 0:qn], in0=o2[:, 0:qn], scalar=nlam[:, 0:1], in1=o1[:, 0:qn], op0=ALU.mult, op1=ALU.add), [o2b, nlamb, o1b], [o1b])
                    k.op("act", lambda e, qn=qn: e.activation(out=sq[:, 0:qn], in_=o1[:, 0:qn], func=AF.Square), [o1b], [sqb])
                    ps, psb = self.psA[n % 2]; n += 1
                    k.op("pe", lambda e, ps=ps, qn=qn: e.matmul(ps[:, 0:qn], lhsT=onesf[:], rhs=sq[:, 0:qn], start=True, stop=True), [onesfb, sqb], [psb])
                    k.op("act", lambda e, ps=ps, qn=qn: e.activation(out=sq[:, 0:qn], in_=ps[:, 0:qn], func=AF.Ln, scale=1.0 / 128, bias=EPS), [psb], [sqb])
                    k.op("act", lambda e, qn=qn: e.activation(out=sq[:, 0:qn], in_=sq[:, 0:qn], func=AF.Exp, scale=-0.5), [sqb], [sqb])
                    k.op("dve", lambda e, qn=qn: e.tensor_tensor(out=o1[:, 0:qn], in0=o1[:, 0:qn], in1=sq[:, 0:qn], op=ALU.mult), [o1b, sqb], [o1b])
                    o_, ob_ = ob[nob % 2]; nob += 1
                    k.op("dve", lambda e, o_=o_, qn=qn: e.tensor_scalar(out=o_[:, 0:qn], in0=o1[:, 0:qn], scalar1=sg[:, 0:1], scalar2=(1.0 - lam_init), op0=ALU.mult, op1=ALU.mult), [o1b, sgb], [ob_])
                    k.dma("sp", lambda e, o_=o_, h=h, q0=q0, qn=qn: e.dma_start(out=self.oT_d[0, h * 128:(h + 1) * 128, q0:q0 + qn], in_=o_[:, 0:qn]), [ob_], [self.oT_b])
        k.es = k.es0
        k.barrier()

    def qk_prep(self, l, b, c0, G, gqk, gqkb, outs):
        k = self.k; inp = self.inp; T, NT, NLT, L = self.T, self.NT, self.NLT, self.L
        W = G * 64
        xq = [k.sb(f"xq{i}", [128, W]) for i in range(2)]
        junk, junkb = k.sb("qjunk", [128, W])
        t2, t2b = k.sb("qt2", [128, W])
        xb16 = [k.sb(f"xb16{i}", [128, W], BF16) for i in range(2)]
        ssq = [k.sb(f"ssq{i}", [128, G]) for i in range(2)]
        cs = [k.sb(f"cs{i}", [128, 64]) for i in range(2)]
        sn = [k.sb(f"sn{i}", [128, 64]) for i in range(2)]
        for tt in range(NT):
            x_, xb_ = xq[tt % 2]; s_, sb_ = ssq[tt % 2]; xo, xob = xb16[tt % 2]
            t0 = tt * 128
            k.dma("sp", lambda e, x_=x_, t0=t0: e.dma_start(out=x_[:], in_=self.proj_d[0, t0:t0 + 128, c0:c0 + W]), [self.proj_b], [xb_])
            k.op("act", lambda e, x_=x_: e.activation(out=junk[:], in_=x_[:], func=AF.Square), [xb_], [junkb])
            k.op("dve", lambda e, s_=s_: e.tensor_reduce(out=s_[:], in_=junk[:].rearrange("p (g c) -> p g c", g=G), axis=AX.X, op=ALU.add), [junkb], [sb_])
            self.rstd(s_[:], sb_, 64)
            x3 = x_[:].rearrange("p (g c) -> p g c", g=G)
            k.op("dve", lambda e, x3=x3, s_=s_: e.tensor_tensor(out=x3, in0=x3, in1=s_[:].unsqueeze(2).to_broadcast([128, G, 64]), op=ALU.mult), [xb_, sb_], [xb_])
            if tt < NLT:
                k.op("pool", lambda e, x3=x3, xo=xo: e.tensor_tensor(out=xo[:].rearrange("p (g c) -> p g c", g=G), in0=x3, in1=gqk[:, 0:G, :], op=ALU.mult), [xb_, gqkb], [xob])
            else:
                c_, cb_ = cs[tt % 2]; n_, nb_ = sn[tt % 2]
                p0 = t0 - L
                k.dma("pool", lambda e, c_=c_, p0=p0: e.dma_start(out=c_[:], in_=inp["rope_cos"][p0:p0 + 128, :]), [], [cb_])
                k.dma("pool", lambda e, n_=n_, p0=p0: e.dma_start(out=n_[:], in_=inp["rope_sin"][p0:p0 + 128, :]), [], [nb_])
                k.op("pool", lambda e, x3=x3: e.tensor_tensor(out=x3, in0=x3, in1=gqk[:, 0:G, :], op=ALU.mult), [xb_, gqkb], [xb_])
                x5 = x_[:].rearrange("p (g a s q) -> p g a s q", g=G, a=2, s=2)
                t5 = t2[:].rearrange("p (g a s q) -> p g a s q", g=G, a=2, s=2)
                n4 = n_[:].rearrange("p (a s q) -> p a s q", a=2, s=2)
                for s in range(2):
                    k.op("dve", lambda e, s=s, x5=x5, t5=t5, n4=n4: e.tensor_tensor(out=t5[:, :, :, s, :], in0=x5[:, :, :, 1 - s, :], in1=n4[:, :, s, :].unsqueeze(1).to_broadcast([128, G, 2, 16]), op=ALU.mult), [xb_, nb_], [t2b])
                k.op("pool", lambda e, x3=x3, c_=c_: e.tensor_tensor(out=x3, in0=x3, in1=c_[:].unsqueeze(1).to_broadcast([128, G, 64]), op=ALU.mult), [xb_, cb_], [xb_])
                k.op("dve", lambda e, x_=x_, xo=xo: e.tensor_tensor(out=xo[:], in0=x_[:], in1=t2[:], op=ALU.add), [xb_, t2b], [xob])
            for (dst, dstb, g0, npair, *wd) in outs:
                wd = wd[0] if wd else 2
                pt, ptb = self.psT[self.ev]
                for j in range(npair):
                    cc = (g0 + wd * j) * 64
                    k.op("pe", lambda e, pt=pt, j=j, cc=cc, xo=xo, wd=wd: e.transpose(out=pt[0:64 * wd, j, :], in_=xo[:, cc:cc + 64 * wd], identity=self.identb[:]), [xob, self.identb_b], [ptb])
                self.evac(dst[0:64 * wd, 0:npair, t0:t0 + 128], pt[0:64 * wd, 0:npair, :], [ptb], [dstb])

    def phase_B(self, l, b):
        k = self.k; inp = self.inp; NB, T, NT, NLT, L, S = self.NB, self.T, self.NT, self.NLT, self.L, self.S
        k.barrier()
        with ExitStack() as es:
            k.es = es
            self.psum_std()
            qT, qTb = k.sb("qTB", [64, 8, T], BF16)
            kT, kTb = k.sb("kTB", [64, 2, T], BF16)
            vB, vBb = k.sb("vB", [128, NT, 128], BF16)
            onesb, onesbb = k.sb("onesb", [128, 64], BF16)
            k.op("pool", lambda e: e.memset(onesb[:], 1.0), [], [onesbb])
            mlo, mlob = k.sb("mlo", [128, 128], BF16); mhi, mhib = k.sb("mhi", [128, 128], BF16)
            mf, mfb = k.sb("mf", [128, 128])
            k.dma("sp", lambda e: e.dma_start(out=mf[:], in_=inp["mask_lo"][:, :]), [], [mfb])
            k.op("dve", lambda e: e.tensor_copy(out=mlo[:], in_=mf[:]), [mfb], [mlob])
            k.dma("sp", lambda e: e.dma_start(out=mf[:], in_=inp["mask_hi"][:, :]), [mlob], [mfb])
            k.op("dve", lambda e: e.tensor_copy(out=mhi[:], in_=mf[:]), [mfb], [mhib])
            sk, skb = k.sb("sk", [128, 8])
            k.dma("sp", lambda e: e.dma_start(out=sk[:], in_=inp["wg_sink"][l, :].partition_broadcast(128)), [], [skb])
            k.op("act", lambda e: e.activation(out=sk[:], in_=sk[:], func=AF.Exp), [skb], [skb])
            gqk, gqkb = k.sb("gqkB", [128, 10, 64])
            for g in range(10):
                k.dma("pool", lambda e, g=g: e.dma_start(out=gqk[:, g, :], in_=inp["qk_norm_g"][l, 2 if g < 8 else 3, :].partition_broadcast(128)), [], [gqkb])
            self.qk_prep(l, b, C_QB, 10, gqk, gqkb, [(qT, qTb, 0, 8, 1), (kT, kTb, 8, 2, 1)])
            vf = [k.sb(f"vfB{i}", [128, 128]) for i in range(2)]
            for tt in range(NT):
                v_, vb_ = vf[tt % 2]
                k.dma("sp", lambda e, v_=v_, tt=tt: e.dma_start(out=v_[:], in_=self.proj_d[0, tt * 128:(tt + 1) * 128, C_VB:C_VB + 128]), [self.proj_b], [vb_])
                k.op("pool", lambda e, v_=v_, tt=tt: e.tensor_copy(out=vB[:, tt, :], in_=v_[:]), [vb_], [vBb])
            pT = [k.sb(f"pTB{i}", [128, 512], BF16) for i in range(3)]
            tot, totb = k.sb("totB", [64, 512])
            ob = [k.sb(f"obB{i}", [64, 512], BF16) for i in range(2)]
            n = 0; nob = 0
            for v in range(2):
                for tt in range(NT):
                    keys = [(kt, None) for kt in range(NLT)]
                    if tt >= NLT:
                        if tt - 1 >= NLT:
                            keys.append((tt - 1, (mlo, mlob)))
                        keys.append((tt, None))
                        if tt + 1 < NT:
                            keys.append((tt + 1, (mhi, mhib)))
                    psO, psOb = self.psA[2 + (nob % 2)]; psS, psSb = self.psA[4 + (nob % 2)]
                    t0 = tt * 128
                    for ki, (kt, msk) in enumerate(keys):
                        ps, psb = self.psA[n % 2]; p_, pb_ = pT[n % 3]; n += 1
                        first = (ki == 0); last = (ki == len(keys) - 1)
                        k.op("pe", lambda e, ps=ps, kt=kt, v=v, t0=t0: e.matmul(ps[:, :].rearrange("p (g q) -> p g q", g=4), lhsT=kT[0:64, v, kt * 128:(kt + 1) * 128], rhs=qT[0:64, 4 * v:4 * v + 4, t0:t0 + 128], start=True, stop=True), [kTb, qTb], [psb])
                        k.op("act", lambda e, ps=ps, p_=p_: e.activation(out=p_[:], in_=ps[:], func=AF.Exp, scale=0.125), [psb], [pb_])
                        if msk is not None:
                            k.op("dve", lambda e, p_=p_, msk=msk: e.tensor_tensor(out=p_[:].rearrange("p (g q) -> p g q", g=4), in0=p_[:].rearrange("p (g q) -> p g q", g=4), in1=msk[0][:].unsqueeze(1).to_broadcast([128, 4, 128]), op=ALU.mult), [pb_, msk[1]], [pb_])
                        k.op("pe", lambda e, psO=psO, kt=kt, v=v, p_=p_, first=first, last=last: e.matmul(psO[0:64, :], lhsT=vB[:, kt, 64 * v:64 * v + 64], rhs=p_[:], start=first, stop=last), [vBb, pb_], [psOb])
                        k.op("pe", lambda e, psS=psS, p_=p_, first=first, last=last: e.matmul(psS[0:64, :], lhsT=onesb[:], rhs=p_[:], start=first, stop=last), [onesbb, pb_], [psSb])
                    k.op("dve", lambda e, psS=psS, v=v: e.tensor_tensor(out=tot[:].rearrange("p (g q) -> p g q", g=4), in0=psS[0:64, :].rearrange("p (g q) -> p g q", g=4), in1=sk[0:64, 4 * v:4 * v + 4].unsqueeze(2).to_broadcast([64, 4, 128]), op=ALU.add), [psSb, skb], [totb])
                    k.op("dve", lambda e: e.reciprocal(out=tot[:], in_=tot[:]), [totb], [totb])
                    o_, ob_ = ob[nob % 2]; nob += 1
                    k.op("dve", lambda e, o_=o_, psO=psO: e.tensor_tensor(out=o_[:], in0=psO[0:64, :], in1=tot[:], op=ALU.mult), [psOb, totb], [ob_])
                    k.dma("sp", lambda e, o_=o_, v=v, t0=t0: e.dma_start(out=self.oT_d[0, 512 + 256 * v:512 + 256 * v + 256, t0:t0 + 128].rearrange("(g d) t -> d g t", g=4), in_=o_[:].rearrange("p (g q) -> p g q", g=4)), [ob_], [self.oT_b])
        k.es = k.es0
        k.barrier()

    def phase_C(self, l, b):
        try:
            self._phase_C(l, b)
        except StopPhase:
            pass
        self.k.es = self.k.es0
        self.k.barrier()

    def cstop(self, n):
        return self.cfg.get("c_stop") == n

    def _phase_C(self, l, b):
        k = self.k; inp = self.inp; NB, T, NT, NLT, L, S = self.NB, self.T, self.NT, self.NLT, self.L, self.S
        k.barrier()
        with ExitStack() as es:
            k.es = es
            banks = [k.ps(f"pc{i}", [128, 512]) for i in range(6)]
            slots = [(banks[i][0][:, 0:128], banks[i][1]) for i in range(6)]
            self.psT = [k.ps(f"psT{i}", [128, 8, 128], BF16) for i in range(2)]
            cnt = [0]
            def PS():
                sl = slots[cnt[0] % 6]; cnt[0] += 1
                return sl
            rings = {}
            def W(name, shape=(128, 128), depth=2):
                if name not in rings:
                    rings[name] = [[k.sb(f"{name}{i}", list(shape)) for i in range(depth)], 0]
                r = rings[name]; r[1] += 1
                return r[0][r[1] % depth]
            ident = self.ident; identb_ = self.ident_b
            gm, gmb = k.sb("gm", [128, 8, 128])
            k.dma("sp", lambda e: e.dma_start(out=gm[:], in_=inp["gmasks"].rearrange("m p f -> p m f")), [], [gmb])
            LE, GE, LT, GT, BD, SEL0, SEL1 = (gm[:, i, :] for i in range(7))
            bias_le, blb = k.sb("bias_le", [128, 128]); bias_ge, bgb = k.sb("bias_ge", [128, 128])
            k.op("dve", lambda e: e.tensor_scalar(out=bias_le[:], in0=LE, scalar1=-1.0, scalar2=30000.0, op0=ALU.add, op1=ALU.mult), [gmb], [blb])
            k.op("dve", lambda e: e.tensor_scalar(out=bias_ge[:], in0=GE, scalar1=-1.0, scalar2=30000.0, op0=ALU.add, op1=ALU.mult), [gmb], [bgb])
            onesf, onesfb = k.sb("onesfC", [128, 128])
            k.op("pool", lambda e: e.memset(onesf[:], 1.0), [], [onesfb])
            araw, arawb = k.sb("araw", [128, NT, 8]); braw, brawb = k.sb("braw", [128, NT, 8])
            g3, g3b = k.sb("g3", [128, NT, 8]); ng3, ng3b = k.sb("ng3", [128, NT, 8])
            be3, be3b = k.sb("be3", [128, NT, 8]); nbe3, nbe3b = k.sb("nbe3", [128, NT, 8])
            tmp3, tmp3b = k.sb("tmp3", [128, NT, 8])
            dtb, dtbb = k.sb("dtb", [128, 8]); nea, neab = k.sb("nea", [128, 8])
            k.dma("sp", lambda e: e.dma_start(out=araw[:], in_=self.proj_d[0, :, C_AC:C_AC + 8].rearrange("(n p) c -> p n c", p=128)), [self.proj_b], [arawb])
            k.dma("sp", lambda e: e.dma_start(out=braw[:], in_=self.proj_d[0, :, C_BC:C_BC + 8].rearrange("(n p) c -> p n c", p=128)), [self.proj_b], [brawb])
            k.dma("pool", lambda e: e.dma_start(out=dtb[:], in_=inp["gd_dt_bias"][l].rearrange("a b -> (a b)").partition_broadcast(128)), [], [dtbb])
            k.dma("pool", lambda e: e.dma_start(out=nea[:], in_=inp["gd_a_log"][l].rearrange("a b -> (a b)").partition_broadcast(128)), [], [neab])
            k.op("act", lambda e: e.activation(out=nea[:], in_=nea[:], func=AF.Exp), [neab], [neab])
            k.op("dve", lambda e: e.tensor_scalar(out=nea[:], in0=nea[:], scalar1=-1.0, scalar2=None, op0=ALU.mult), [neab], [neab])
            k.op("dve", lambda e: e.tensor_tensor(out=araw[:], in0=araw[:], in1=dtb[:].unsqueeze(1).to_broadcast([128, NT, 8]), op=ALU.add), [arawb, dtbb], [arawb])
            k.op("act", lambda e: e.activation(out=tmp3[:], in_=araw[:], func=AF.Abs), [arawb], [tmp3b])
            k.op("act", lambda e: e.activation(out=tmp3[:], in_=tmp3[:], func=AF.Exp, scale=-1.0), [tmp3b], [tmp3b])
            k.op("act", lambda e: e.activation(out=tmp3[:], in_=tmp3[:], func=AF.Ln, bias=1.0), [tmp3b], [tmp3b])
            k.op("dve", lambda e: e.scalar_tensor_tensor(out=tmp3[:], in0=araw[:], scalar=0.0, in1=tmp3[:], op0=ALU.max, op1=ALU.add), [arawb, tmp3b], [tmp3b])
            k.op("dve", lambda e: e.tensor_tensor(out=g3[:], in0=tmp3[:], in1=nea[:].unsqueeze(1).to_broadcast([128, NT, 8]), op=ALU.mult), [tmp3b, neab], [g3b])
            k.op("dve", lambda e: e.tensor_scalar(out=ng3[:], in0=g3[:], scalar1=-1.0, scalar2=None, op0=ALU.mult), [g3b], [ng3b])
            k.op("act", lambda e: e.activation(out=be3[:], in_=braw[:], func=AF.Sigmoid), [brawb], [be3b])
            k.op("dve", lambda e: e.tensor_scalar(out=nbe3[:], in0=be3[:], scalar1=-1.0, scalar2=None, op0=ALU.mult), [be3b], [nbe3b])
            if self.cstop(1):
                return
            xin, xinb = k.sb("xin", [128, T]); ybuf, ybufb = k.sb("ybuf", [128, T])
            qT, qTb = k.sb("qTC", [128, T]); kT, kTb = k.sb("kTC", [128, T])
            k_tm, k_tmb = k.sb("k_tm", [128, NT, 128]); v_tm, v_tmb = k.sb("v_tm", [128, NT, 128])
            o_acc, o_accb = k.sb("o_acc", [128, NT, 128])
            S_, Sb = k.sb("Sstate", [128, 128])
            cw, cwb = k.sb("cw", [128, 5])
            gng, gngb = k.sb("gng", [128, 128])
            k.dma("pool", lambda e: e.dma_start(out=gng[:], in_=inp["gd_norm_g"][l, :].partition_broadcast(128)), [], [gngb])
            segs = [(0, L), (L, T)]
            for h in range(4):
                for part, (dst, dstb) in enumerate(((qT, qTb), (kT, kTb), (ybuf, ybufb))):
                    ch0 = (part * 4 + h) * 128
                    k.dma("sp", lambda e, ch0=ch0: e.dma_start(out=xin[:], in_=self.qkvT_d[0, ch0:ch0 + 128, :]), [self.qkvT_b], [xinb])
                    k.dma("pool", lambda e, ch0=ch0: e.dma_start(out=cw[:], in_=inp["gd_conv_w"][l, :, ch0:ch0 + 128].rearrange("j c -> c j"), allow_slow_non_contiguous=True), [], [cwb])
                    k.op("dve", lambda e, dst=dst: e.tensor_scalar(out=dst[:], in0=xin[:], scalar1=cw[:, 2:3], scalar2=None, op0=ALU.mult), [xinb, cwb], [dstb])
                    for j in (0, 1, 3, 4):
                        d = j - 2
                        for (a, z) in segs:
                            lo, hi = (a, z - d) if d > 0 else (a - d, z)
                            k.op("dve", lambda e, dst=dst, lo=lo, hi=hi, d=d, j=j: e.scalar_tensor_tensor(out=dst[:, lo:hi], in0=xin[:, lo + d:hi + d], scalar=cw[:, j:j + 1], in1=dst[:, lo:hi], op0=ALU.mult, op1=ALU.add), [xinb, cwb, dstb], [dstb])
                    k.op("act", lambda e, dst=dst: e.activation(out=dst[:], in_=dst[:], func=AF.Silu), [dstb], [dstb])
                    if self.cstop(5):
                        return
                    if part < 2:
                        k.op("act", lambda e, dst=dst: e.activation(out=xin[:], in_=dst[:], func=AF.Square), [dstb], [xinb])
                        for c0 in range(0, T, 512):
                            cn = min(512, T - c0)
                            bi = (c0 // 512) % 2
                            ps = banks[bi][0]; bbufs = [banks[bi][1]]
                            k.op("pe", lambda e, ps=ps, c0=c0, cn=cn: e.matmul(ps[:, 0:cn], lhsT=onesf[:], rhs=xin[:, c0:c0 + cn], start=True, stop=True), [onesfb, xinb], bbufs)
                            k.op("act", lambda e, ps=ps, c0=c0, cn=cn: e.activation(out=xin[:, c0:c0 + cn], in_=ps[:, 0:cn], func=AF.Ln, bias=EPS), bbufs + [xinb], [xinb])
                        k.op("act", lambda e: e.activation(out=xin[:], in_=xin[:], func=AF.Exp, scale=-0.5), [xinb], [xinb])
                        sc = (128.0 ** -0.5) if part == 0 else 1.0
                        k.op("dve", lambda e, dst=dst, sc=sc: e.scalar_tensor_tensor(out=dst[:], in0=dst[:], scalar=sc, in1=xin[:], op0=ALU.mult, op1=ALU.mult), [dstb, xinb], [dstb])
                    if part >= 1:
                        if self.cstop(60 + part):
                            return
                        tm, tmb = (k_tm, k_tmb) if part == 1 else (v_tm, v_tmb)
                        for tt in range(NT):
                            ps, psb = PS()
                            k.op("pe", lambda e, ps=ps, dst=dst, tt=tt: e.matmul(ps, lhsT=dst[:, tt * 128:(tt + 1) * 128], rhs=ident[:], start=True, stop=True), [dstb, identb_], [psb])
                            self.evac(tm[:, tt, :], ps, [psb], [tmb])
                        if self.cstop(70 + part):
                            return
                if self.cstop(2):
                    return
                k.op("pool", lambda e: e.memset(o_acc[:], 0.0), [], [o_accb])
                for d in range(2):
                    c = d * 4 + h
                    Linc = LE if d == 0 else GE
                    bias_i, bias_ib = (bias_ge, bgb) if d == 0 else (bias_le, blb)
                    St = GT if d == 0 else LT
                    bias_t, bias_tb = (bias_le, blb) if d == 0 else (bias_ge, bgb)
                    k.op("pool", lambda e: e.memset(S_[:], 0.0), [], [Sb])
                    order = list(range(NT)) if d == 0 else (list(range(NLT - 1, -1, -1)) + list(range(NT - 1, NLT - 1, -1)))
                    for tt in order:
                        t0 = tt * 128
                        gcol = g3[:, tt, c:c + 1]; ngcol = ng3[:, tt, c:c + 1]
                        G, Gb = W("G"); nG, nGb = W("nG")
                        k.op("dve", lambda e, G=G, gcol=gcol, Linc=Linc: e.tensor_scalar(out=G[:], in0=Linc, scalar1=gcol, scalar2=None, op0=ALU.mult), [gmb, g3b], [Gb])
                        k.op("pool", lambda e, nG=nG, ngcol=ngcol, Linc=Linc: e.tensor_scalar(out=nG[:], in0=Linc, scalar1=ngcol, scalar2=None, op0=ALU.mult), [gmb, ng3b], [nGb])
                        pD, pDb = PS(); pDT, pDTb = PS(); pg, pgb = PS()
                        k.op("pe", lambda e, pD=pD, G=G: e.matmul(pD, lhsT=G[:], rhs=onesf[:], start=True, stop=False), [Gb, onesfb], [pDb])
                        k.op("pe", lambda e, pD=pD, nG=nG: e.matmul(pD, lhsT=onesf[:], rhs=nG[:], start=False, stop=True), [nGb, onesfb], [pDb])
                        k.op("pe", lambda e, pDT=pDT, G=G: e.matmul(pDT, lhsT=onesf[:], rhs=G[:], start=True, stop=False), [Gb, onesfb], [pDTb])
                        k.op("pe", lambda e, pDT=pDT, nG=nG: e.matmul(pDT, lhsT=nG[:], rhs=onesf[:], start=False, stop=True), [nGb, onesfb], [pDTb])
                        for ci, msk in enumerate((Linc, BD, SEL0, SEL1)):
                            k.op("pe", lambda e, pg=pg, ci=ci, msk=msk, gcol=gcol: e.matmul(pg[:, ci:ci + 1], lhsT=msk, rhs=gcol, start=True, stop=True), [gmb, g3b], [pgb])
                        sc4, sc4b = W("sc4", (128, 8))
                        k.op("dve", lambda e, sc4=sc4, pg=pg: e.tensor_copy(out=sc4[:, 7:8], in_=pg[:, 0:1]), [pgb], [sc4b])
                        k.op("act", lambda e, sc4=sc4, pg=pg: e.activation(out=sc4[:, 0:1], in_=pg[:, 0:1], func=AF.Exp), [pgb], [sc4b])
                        k.op("dve", lambda e, sc4=sc4, pg=pg: e.tensor_tensor(out=sc4[:, 1:2], in0=pg[:, 1:2], in1=sc4[:, 7:8], op=ALU.subtract), [pgb, sc4b], [sc4b])
                        k.op("act", lambda e, sc4=sc4: e.activation(out=sc4[:, 1:2], in_=sc4[:, 1:2], func=AF.Exp), [sc4b], [sc4b])
                        k.op("act", lambda e, sc4=sc4, pg=pg: e.activation(out=sc4[:, 2:4], in_=pg[:, 2:4], func=AF.Exp), [pgb], [sc4b])
                        k.op("dve", lambda e, sc4=sc4, tt=tt, c=c: e.tensor_tensor(out=sc4[:, 4:5], in0=sc4[:, 0:1], in1=be3[:, tt, c:c + 1], op=ALU.mult), [sc4b, be3b], [sc4b])
                        k.op("dve", lambda e, sc4=sc4: e.tensor_tensor(out=sc4[:, 5:6], in0=sc4[:, 1:2], in1=SEL0[:, 0:1], op=ALU.mult), [sc4b, gmb], [sc4b])
                        k.op("dve", lambda e, sc4=sc4: e.tensor_tensor(out=sc4[:, 6:7], in0=sc4[:, 1:2], in1=SEL1[:, 0:1], op=ALU.mult), [sc4b, gmb], [sc4b])
                        dec, decb = W("dec"); decS, decSb = W("decS"); decT, decTb = W("decT")
                        k.op("dve", lambda e, dec=dec, pD=pD, bias_i=bias_i: e.tensor_tensor(out=dec[:], in0=pD, in1=bias_i[:], op=ALU.add), [pDb, bias_ib], [decb])
                        k.op("act", lambda e, dec=dec: e.activation(out=dec[:], in_=dec[:], func=AF.Exp), [decb], [decb])
                        k.op("pool", lambda e, dec=dec, decS=decS, St=St: e.tensor_tensor(out=decS[:], in0=dec[:], in1=St, op=ALU.mult), [decb, gmb], [decSb])
                        k.op("dve", lambda e, decT=decT, pDT=pDT, bias_t=bias_t: e.tensor_tensor(out=decT[:], in0=pDT, in1=bias_t[:], op=ALU.add), [pDTb, bias_tb], [decTb])
                        k.op("act", lambda e, decT=decT: e.activation(out=decT[:], in_=decT[:], func=AF.Exp), [decTb], [decTb])
                        pKK, pKKb = PS(); pQK, pQKb = PS()
                        k.op("pe", lambda e, pKK=pKK, t0=t0: e.matmul(pKK, lhsT=kT[:, t0:t0 + 128], rhs=kT[:, t0:t0 + 128], start=True, stop=True), [kTb], [pKKb])
                        k.op("pe", lambda e, pQK=pQK, t0=t0: e.matmul(pQK, lhsT=kT[:, t0:t0 + 128], rhs=qT[:, t0:t0 + 128], start=True, stop=True), [kTb, qTb], [pQKb])
                        Q, Qb = W("Q", depth=3); P, Pb = W("P", depth=3); TT, TTb = W("TT"); qkmT, qkmTb = W("qkmT")
                        k.op("dve", lambda e, Q=Q, pKK=pKK, decS=decS, tt=tt, c=c: e.scalar_tensor_tensor(out=Q[:], in0=pKK, scalar=nbe3[:, tt, c:c + 1], in1=decS[:], op0=ALU.mult, op1=ALU.mult), [pKKb, nbe3b, decSb], [Qb])
                        k.op("dve", lambda e, qkmT=qkmT, pQK=pQK, decT=decT: e.tensor_tensor(out=qkmT[:], in0=pQK, in1=decT[:], op=ALU.mult), [pQKb, decTb], [qkmTb])
                        pP, pPb = PS()
                        k.op("pe", lambda e, pP=pP, Q=Q: e.matmul(pP, lhsT=Q[:], rhs=ident[:], start=True, stop=True), [Qb, identb_], [pPb])
                        k.op("act", lambda e, P=P, pP=pP: e.activation(out=P[:], in_=pP, func=AF.Copy), [pPb], [Pb])
                        k.op("dve", lambda e, TT=TT, pP=pP: e.tensor_tensor(out=TT[:], in0=pP, in1=ident[:], op=ALU.add), [pPb, identb_], [TTb])
                        for lev in range(5):
                            Qn, Qnb = W("Q", depth=3); pQn, pQnb = PS()
                            k.op("pe", lambda e, pQn=pQn, P=P, Q=Q: e.matmul(pQn, lhsT=P[:], rhs=Q[:], start=True, stop=True), [Pb, Qb], [pQnb])
                            if lev < 4:
                                Pn, Pnb = W("P", depth=3); pPn, pPnb = PS()
                                k.op("pe", lambda e, pPn=pPn, P=P, Q=Q: e.matmul(pPn, lhsT=Q[:], rhs=P[:], start=True, stop=True), [Pb, Qb], [pPnb])
                                k.op("act", lambda e, Pn=Pn, pPn=pPn: e.activation(out=Pn[:], in_=pPn, func=AF.Copy), [pPnb], [Pnb])
                            k.op("dve", lambda e, Qn=Qn, pQn=pQn: e.tensor_copy(out=Qn[:], in_=pQn), [pQnb], [Qnb])
                            pU, pUb = PS()
                            k.op("pe", lambda e, pU=pU, Qn=Qn, TT=TT: e.matmul(pU, lhsT=Qn[:], rhs=TT[:], start=True, stop=True), [Qnb, TTb], [pUb])
                            k.op("dve", lambda e, TT=TT, pU=pU: e.tensor_tensor(out=TT[:], in0=TT[:], in1=pU, op=ALU.add), [TTb, pUb], [TTb])
                            Q, Qb = Qn, Qnb
                            if lev < 4:
                                P, Pb = Pn, Pnb
                        if self.cstop(3):
                            return
                        vb, vbb = W("vb"); kbg, kbgb = W("kbg"); kd0, kd0b = W("kd0"); kd1, kd1b = W("kd1")
                        k.op("pool", lambda e, vb=vb, tt=tt, c=c: e.tensor_scalar(out=vb[:], in0=v_tm[:, tt, :], scalar1=be3[:, tt, c:c + 1], scalar2=None, op0=ALU.mult), [v_tmb, be3b], [vbb])
                        k.op("pool", lambda e, kbg=kbg, tt=tt, sc4=sc4: e.tensor_scalar(out=kbg[:], in0=k_tm[:, tt, :], scalar1=sc4[:, 4:5], scalar2=None, op0=ALU.mult), [k_tmb, sc4b], [kbgb])
                        k.op("pool", lambda e, kd0=kd0, tt=tt, sc4=sc4: e.tensor_scalar(out=kd0[:], in0=k_tm[:, tt, :], scalar1=sc4[:, 5:6], scalar2=None, op0=ALU.mult), [k_tmb, sc4b], [kd0b])
                        k.op("pool", lambda e, kd1=kd1, tt=tt, sc4=sc4: e.tensor_scalar(out=kd1[:], in0=k_tm[:, tt, :], scalar1=sc4[:, 6:7], scalar2=None, op0=ALU.mult), [k_tmb, sc4b], [kd1b])
                        pu, pub = PS(); pw, pwb = PS()
                        k.op("pe", lambda e, pu=pu, TT=TT, vb=vb: e.matmul(pu, lhsT=TT[:], rhs=vb[:], start=True, stop=True), [TTb, vbb], [pub])
                        k.op("pe", lambda e, pw=pw, TT=TT, kbg=kbg: e.matmul(pw, lhsT=kbg[:], rhs=TT[:], start=True, stop=True), [TTb, kbgb], [pwb])
                        u, ub = W("u"); wT, wTb = W("wT")
                        k.op("act", lambda e, u=u, pu=pu: e.activation(out=u[:], in_=pu, func=AF.Copy), [pub], [ub])
                        k.op("dve", lambda e, wT=wT, pw=pw: e.tensor_copy(out=wT[:], in_=pw), [pwb], [wTb])
                        for ch in ((0, 1) if d == 0 else (1, 0)):
                            kd, kdb = (kd0, kd0b) if ch == 0 else (kd1, kd1b)
                            cm = (SEL0 if ch == 0 else SEL1)[:, 0:1]
                            pwS, pwSb = PS(); pqS, pqSb = PS(); po1, po1b = PS(); pSn, pSnb = PS()
                            k.op("pe", lambda e, pwS=pwS, wT=wT: e.matmul(pwS, lhsT=wT[:], rhs=S_[:], start=True, stop=True), [wTb, Sb], [pwSb])
                            k.op("pe", lambda e, pqS=pqS, t0=t0: e.matmul(pqS, lhsT=qT[:, t0:t0 + 128], rhs=S_[:], start=True, stop=True), [qTb, Sb], [pqSb])
                            vn, vnb = W("vn"); tq, tqb = W("tq")
                            k.op("dve", lambda e, vn=vn, u=u, pwS=pwS: e.tensor_tensor(out=vn[:], in0=u[:], in1=pwS, op=ALU.subtract), [ub, pwSb], [vnb])
                            k.op("pe", lambda e, po1=po1, qkmT=qkmT, vn=vn: e.matmul(po1, lhsT=qkmT[:], rhs=vn[:], start=True, stop=True), [qkmTb, vnb], [po1b])
                            k.op("pe", lambda e, pSn=pSn, kd=kd, vn=vn: e.matmul(pSn, lhsT=kd[:], rhs=vn[:], start=True, stop=True), [kdb, vnb], [pSnb])
                            k.op("act", lambda e, tq=tq, pqS=pqS, sc4=sc4: e.activation(out=tq[:], in_=pqS, func=AF.Copy, scale=sc4[:, 0:1]), [pqSb, sc4b], [tqb])
                            k.op("dve", lambda e, tq=tq, po1=po1: e.tensor_tensor(out=tq[:], in0=tq[:], in1=po1, op=ALU.add), [tqb, po1b], [tqb])
                            k.op("dve", lambda e, tq=tq, cm=cm, tt=tt: e.scalar_tensor_tensor(out=o_acc[:, tt, :], in0=tq[:], scalar=cm, in1=o_acc[:, tt, :], op0=ALU.mult, op1=ALU.add), [tqb, gmb, o_accb], [o_accb])
                            k.op("dve", lambda e, pSn=pSn, sc4=sc4, ch=ch: e.scalar_tensor_tensor(out=S_[:], in0=S_[:], scalar=sc4[:, 2 + ch:3 + ch], in1=pSn, op0=ALU.mult, op1=ALU.add), [Sb, sc4b, pSnb], [Sb])
                if self.cstop(4):
                    return
                zt = xin[:, 0:NT * 128].rearrange("p (n c) -> p n c", c=128)
                k.dma("sp", lambda e, h=h: e.dma_start(out=zt, in_=self.proj_d[0, :, C_ZC + h * 128:C_ZC + (h + 1) * 128].rearrange("(n p) c -> p n c", p=128)), [self.proj_b], [xinb])
                k.op("act", lambda e: e.activation(out=zt, in_=zt, func=AF.Silu), [xinb], [xinb])
                sq3 = ybuf[:, 0:NT * 128].rearrange("p (n c) -> p n c", c=128)
                ssn, ssnb = W("ssn", (128, NT), depth=1)
                k.op("act", lambda e: e.activation(out=sq3, in_=o_acc[:], func=AF.Square), [o_accb], [ybufb])
                k.op("dve", lambda e, ssn=ssn: e.tensor_reduce(out=ssn[:], in_=sq3, axis=AX.X, op=ALU.add), [ybufb], [ssnb])
                self.rstd(ssn[:], ssnb, 128)
                k.op("dve", lambda e, ssn=ssn: e.tensor_tensor(out=o_acc[:], in0=o_acc[:], in1=ssn[:].unsqueeze(2).to_broadcast([128, NT, 128]), op=ALU.mult), [o_accb, ssnb], [o_accb])
                k.op("pool", lambda e: e.tensor_tensor(out=o_acc[:], in0=o_acc[:], in1=gng[:].unsqueeze(1).to_broadcast([128, NT, 128]), op=ALU.mult), [o_accb, gngb], [o_accb])
                ob16 = kT[:].bitcast(BF16)[:, 0:NT * 128].rearrange("p (n c) -> p n c", c=128)
                k.op("dve", lambda e, ob16=ob16: e.tensor_tensor(out=ob16, in0=o_acc[:], in1=zt, op=ALU.mult), [o_accb, xinb], [kTb])
                oT16 = qT[:].bitcast(BF16)[:, 0:T]
                for t8 in range(0, NT, 8):
                    n8 = min(8, NT - t8)
                    pt, ptb = self.psT[(t8 // 8) % 2]
                    for j in range(n8):
                        k.op("pe", lambda e, pt=pt, j=j, t8=t8, ob16=ob16: e.transpose(out=pt[:, j, :], in_=ob16[:, t8 + j, :], identity=self.identb[:]), [kTb, self.identb_b], [ptb])
                    self.evac(oT16[:, t8 * 128:(t8 + n8) * 128].rearrange("p (n c) -> p n c", c=128), pt[:, 0:n8, :], [ptb], [qTb])
                k.dma("sp", lambda e, h=h, oT16=oT16: e.dma_start(out=self.oT_d[0, 1024 + h * 128:1024 + (h + 1) * 128, :], in_=oT16), [qTb], [self.oT_b])
        k.es = k.es0
        k.barrier()

    def load_w16(self, dst16, dstb, src_ap_fn, ncols, stage):
        k = self.k
        for i, c0 in enumerate(range(0, ncols, 256)):
            cw = min(256, ncols - c0)
            s_, sb_ = stage[i % len(stage)]
            k.dma("sp", lambda e, s_=s_, c0=c0, cw=cw: e.dma_start(out=s_[:, :, 0:cw], in_=src_ap_fn(c0, cw)), [], [sb_])
            k.op("pool", lambda e, s_=s_, c0=c0, cw=cw: e.tensor_copy(out=dst16[:, :, c0:c0 + cw], in_=s_[:, :, 0:cw]), [sb_], [dstb])

    def phase_M(self, l, b):
        k = self.k; inp = self.inp; NB, T, NT, NLT = self.NB, self.T, self.NT, self.NLT
        last = (l == self.DEPTH - 1)
        k.barrier()
        with ExitStack() as es:
            k.es = es
            self.psum_std()
            stage = [k.sb(f"stg{i}", [128, 8, 256]) for i in range(2)]
            wbr, wbrb = k.sb("wbr", [128, 12, D], BF16)
            wout, woutb = k.sb("wout", [128, 8, D], BF16)
            for r in range(3):
                for c in range(4):
                    pass
            st4 = [k.sb(f"stg4{i}", [128, 4, 256]) for i in range(2)]
            n = 0
            for r in range(3):
                for c0 in range(0, D, 256):
                    s_, sb_ = st4[n % 2]; n += 1
                    k.dma("sp", lambda e, s_=s_, r=r, c0=c0: e.dma_start(out=s_[:], in_=inp["w_branch"][l, r, :, c0:c0 + 256].rearrange("(c p) d -> p c d", p=128)), [], [sb_])
                    k.op("pool", lambda e, s_=s_, r=r, c0=c0: e.tensor_copy(out=wbr[:, 4 * r:4 * r + 4, c0:c0 + 256], in_=s_[:]), [sb_], [wbrb])
            self.load_w16(wout, woutb, lambda c0, cw: inp["w_out"][l, :, c0:c0 + cw].rearrange("(k p) c -> p k c", p=128), D, stage)
            bg, bgb = k.sb("bgate", [128, 3 * D])
            k.dma("sp", lambda e: e.dma_start(out=bg[:], in_=inp["b_gate"][l, :].partition_broadcast(128)), [], [bgb])
            gt1, gt1b = k.sb("gt1", [128, D])
            gl, glb = k.sb("gl", [128, 3 * D])
            oT = [k.sb(f"oTm{i}", [128, 12, 128], BF16) for i in range(2)]
            xt, xtb = k.sb("xm", [128, D]); mp, mpb = k.sb("mp", [128, D]); tmpm, tmpmb = k.sb("tmpm", [128, D])
            mp16, mp16b = k.sb("mp16", [128, D], BF16); mT, mTb = k.sb("mTm", [128, 8, 128], BF16)
            cur_kind = None
            for tt in range(NT):
                kind = "ctx" if tt < NLT else "lat"
                if kind == "ctx" and last:
                    continue
                if kind != cur_kind:
                    cur_kind = kind
                    r_ = NB if kind == "ctx" else b
                    k.dma("sp", lambda e, r_=r_: e.dma_start(out=gt1[:], in_=self.mod_d[r_, 2 * D:3 * D].partition_broadcast(128)), [self.mod_b], [gt1b])
                t0 = tt * 128
                o_, ob_ = oT[tt % 2]
                k.dma("sp", lambda e, o_=o_, t0=t0: e.dma_start(out=o_[:], in_=self.oT_d[0, :, t0:t0 + 128].rearrange("(c p) t -> p c t", p=128)), [self.oT_b], [ob_])
                k.dma("pool", lambda e, t0=t0: e.dma_start(out=gl[:], in_=self.proj_d[0, t0:t0 + 128, C_GL:C_GL + 3 * D]), [self.proj_b], [glb])
                k.dma("sp", lambda e, t0=t0: e.dma_start(out=xt[:], in_=self.xs[b, t0:t0 + 128, :]), [self.xs_b], [xtb])
                k.op("pool", lambda e: e.tensor_tensor(out=gl[:], in0=gl[:], in1=bg[:], op=ALU.add), [glb, bgb], [glb])
                k.op("act", lambda e: e.activation(out=gl[:], in_=gl[:], func=AF.Sigmoid), [glb], [glb])
                for r in range(3):
                    for hf in range(2):
                        ps, psb = self.psA[r * 2 + hf]
                        for c in range(4):
                            k.op("pe", lambda e, ps=ps, r=r, c=c, hf=hf, o_=o_: e.matmul(ps[:], lhsT=o_[:, 4 * r + c, :], rhs=wbr[:, 4 * r + c, hf * 512:(hf + 1) * 512], start=(c == 0), stop=(c == 3)), [ob_, wbrb], [psb])
                for hf in range(2):
                    sl = slice(hf * 512, (hf + 1) * 512)
                    for r in range(3):
                        ps, psb = self.psA[r * 2 + hf]
                        dst, dstb = (mp, mpb) if r == 0 else (tmpm, tmpmb)
                        k.op("dve", lambda e, ps=ps, dst=dst, r=r, sl=sl: e.tensor_tensor(out=dst[:, sl], in0=ps[:], in1=gl[:, r * D + sl.start:r * D + sl.stop], op=ALU.mult), [psb, glb], [dstb])
                        if r > 0:
                            k.op("pool", lambda e, sl=sl: e.tensor_tensor(out=mp[:, sl], in0=mp[:, sl], in1=tmpm[:, sl], op=ALU.add), [mpb, tmpmb], [mpb])
                k.op("act", lambda e: e.activation(out=mp16[:], in_=mp[:], func=AF.Copy), [mpb], [mp16b])
                pt, ptb = self.psT[tt % 2]
                for kk in range(KD):
                    k.op("pe", lambda e, pt=pt, kk=kk: e.transpose(out=pt[:, kk, :], in_=mp16[:, kk * 128:(kk + 1) * 128], identity=self.identb[:]), [mp16b, self.identb_b], [ptb])
                self.evac(mT[:], pt[:], [ptb], [mTb])
                for hf in range(2):
                    sl = slice(hf * 512, (hf + 1) * 512)
                    ps, psb = self.psA[hf]
                    for kk in range(KD):
                        k.op("pe", lambda e, ps=ps, kk=kk, sl=sl: e.matmul(ps[:], lhsT=mT[:, kk, :], rhs=wout[:, kk, sl], start=(kk == 0), stop=(kk == KD - 1)), [mTb, woutb], [psb])
                    k.op("dve", lambda e, ps=ps, sl=sl: e.tensor_tensor(out=tmpm[:, sl], in0=ps[:], in1=gt1[:, sl], op=ALU.mult), [psb, gt1b], [tmpmb])
                    k.op("pool", lambda e, sl=sl: e.tensor_tensor(out=xt[:, sl], in0=xt[:, sl], in1=tmpm[:, sl], op=ALU.add), [xtb, tmpmb], [xtb])
                k.dma("sp", lambda e, t0=t0: e.dma_start(out=self.xs[b, t0:t0 + 128, :], in_=xt[:]), [xtb], [self.xs_b])
        k.es = k.es0
        k.barrier()

    def phase_P(self, l, b):
        k = self.k; inp = self.inp; NB, T, NT, NLT = self.NB, self.T, self.NT, self.NLT
        last = (l == self.DEPTH - 1)
        k.barrier()
        with ExitStack() as es:
            k.es = es
            self.psum_std()
            stage = [k.sb(f"stgp{i}", [128, 8, 256]) for i in range(2)]
            wq, wqb = k.sb("wq", [128, 8, 2048], BF16)
            self.load_w16(wq, wqb, lambda c0, cw: inp["peer_wq"][l, :, c0:c0 + cw].rearrange("(k p) c -> p k c", p=128), 2048, stage)
            keysT, keysTb = k.sb("keysT", [128, 2, 128])
            kn, knb = k.sb("keysN", [128, 2, 128])
            k.dma("pool", lambda e: e.dma_start(out=kn[:], in_=inp["peer_keys"][l].rearrange("f n d -> n f d")), [], [knb])
            for hf in range(2):
                ps, psb = self.psA[hf]
                k.op("pe", lambda e, ps=ps, hf=hf: e.matmul(ps[:, 0:128], lhsT=kn[:, hf, :], rhs=self.ident[:], start=True, stop=True), [knb, self.ident_b], [psb])
                self.evac(keysT[:, hf, :], ps[:, 0:128], [psb], [keysTb])
            A2, A2b = k.sb("A2", [128, D]); g2, g2b = k.sb("g2", [128, D]); B2, B2b = k.sb("B2", [128, D]); gt2, gt2b = k.sb("gt2", [128, D])
            k.dma("pool", lambda e: e.dma_start(out=g2[:], in_=inp["norm2_g"][l, :].partition_broadcast(128)), [], [g2b])
            iot, iotb = k.sb("iota16", [128, 16])
            k.op("pool", lambda e: e.iota(iot[:], pattern=[[1, 16]], base=0, channel_multiplier=0, allow_small_or_imprecise_dtypes=True), [], [iotb])
            xt, xtb = k.sb("xp", [128, D]); junk, junkb = k.sb("junkp", [128, D]); ss, ssb = k.sb("ssp", [128, 1])
            h2, h2b = k.sb("h2", [128, D]); h16, h16b = k.sb("h16", [128, D], BF16); h2T, h2Tb = k.sb("h2T", [128, 8, 128], BF16)
            qTs, qTsb = k.sb("qTs", [128, 16, 128]); sc, scb = k.sb("sc", [128, 16, 128]); wk, wkb = k.sb("wk", [128, 16, 128])
            m16, m16b = k.sb("m16", [128, 16, 16]); ix, ixb = k.sb("ix", [128, 16, 16], U32); ixf, ixfb = k.sb("ixf", [128, 16, 16])
            cand, candb = k.sb("cand", [128, 8, 256])
            cwk = qTs[:].rearrange("p g n -> p (g n)").rearrange("p (h c) -> p h c", h=8); cwkb = qTsb
            cv, cvb = k.sb("cv", [128, 8, 16]); ci, cib = k.sb("ci", [128, 8, 16], U32); cj, cjb = k.sb("cj", [128, 8, 16], U32)
            cif, cifb = k.sb("cif", [128, 8, 16]); cjf, cjfb = k.sb("cjf", [128, 8, 16])
            i1, i1b = k.sb("i1", [128, 8, 16]); i2, i2b = k.sb("i2", [128, 8, 16])
            E = wk[:].rearrange("p g n -> p (g n)").rearrange("p (h k i) -> p h k i", h=8, k=16); Eb = wkb
            eid, eidb = k.sb("eid", [128, 128], I32)
            gw, gwb = k.sb("gw", [128, 8, 16]); gs, gsb = k.sb("gs", [128, 8]); act, actb = k.sb("actp", [128, 128])
            NG = 14
            gbuf = [k.sb(f"gath{i}", [128, 2 * D], BF16) for i in range(NG)]
            wact, wactb = k.sb("wact", [128, 128])
            junks = [(junk, junkb)] + [k.sb(f"junkp{i}", [128, D]) for i in range(1)]
            accs = [k.sb(f"accs{i}", [128, D]) for i in range(2)]
            acc, accb = k.sb("accp", [128, D])
            cur_kind = None; ng = 0
            for tt in range(NT):
                kind = "ctx" if tt < NLT else "lat"
                if kind == "ctx" and last:
                    continue
                if kind != cur_kind:
                    cur_kind = kind
                    r_ = NB if kind == "ctx" else b
                    k.dma("sp", lambda e, r_=r_: e.dma_start(out=A2[:], in_=self.mod_d[r_, 4 * D:5 * D].partition_broadcast(128)), [self.mod_b], [A2b])
                    k.op("dve", lambda e: e.scalar_tensor_tensor(out=A2[:], in0=A2[:], scalar=1.0, in1=g2[:], op0=ALU.add, op1=ALU.mult), [A2b, g2b], [A2b])
                    k.dma("sp", lambda e, r_=r_: e.dma_start(out=B2[:], in_=self.mod_d[r_, 3 * D:4 * D].partition_broadcast(128)), [self.mod_b], [B2b])
                    k.dma("sp", lambda e, r_=r_: e.dma_start(out=gt2[:], in_=self.mod_d[r_, 5 * D:6 * D].partition_broadcast(128)), [self.mod_b], [gt2b])
                t0 = tt * 128
                k.dma("sp", lambda e, t0=t0: e.dma_start(out=xt[:], in_=self.xs[b, t0:t0 + 128, :]), [self.xs_b], [xtb])
                k.op("act", lambda e: e.activation(out=junk[:], in_=xt[:], func=AF.Square, accum_out=ss[:]), [xtb], [junkb, ssb])
                self.rstd(ss[:], ssb, D)
                k.op("dve", lambda e: e.scalar_tensor_tensor(out=h2[:], in0=xt[:], scalar=ss[:, 0:1], in1=A2[:], op0=ALU.mult, op1=ALU.mult), [xtb, ssb, A2b], [h2b])
                k.op("pool", lambda e: e.tensor_tensor(out=h2[:], in0=h2[:], in1=B2[:], op=ALU.add), [h2b, B2b], [h2b])
                k.op("act", lambda e: e.activation(out=h16[:], in_=h2[:], func=AF.Copy), [h2b], [h16b])
                pt, ptb = self.psT[tt % 2]
                for kk in range(KD):
                    k.op("pe", lambda e, pt=pt, kk=kk: e.transpose(out=pt[:, kk, :], in_=h16[:, kk * 128:(kk + 1) * 128], identity=self.identb[:]), [h16b, self.identb_b], [ptb])
                self.evac(h2T[:], pt[:], [ptb], [h2Tb])
                for g in range(16):
                    ps, psb = self.psA[g // 4]
                    for kk in range(KD):
                        k.op("pe", lambda e, ps=ps, g=g, kk=kk: e.matmul(ps[:, (g % 4) * 128:(g % 4 + 1) * 128], lhsT=wq[:, kk, g * 128:(g + 1) * 128], rhs=h2T[:, kk, :], start=(kk == 0), stop=(kk == KD - 1)), [wqb, h2Tb], [psb])
                    if g % 4 == 3:
                        self.evac(qTs[:, g - 3:g + 1, :], ps[:].rearrange("p (g q) -> p g q", g=4), [psb], [qTsb])
                for g in range(16):
                    ps, psb = self.psA[g // 4]
                    k.op("pe", lambda e, ps=ps, g=g: e.matmul(ps[:, (g % 4) * 128:(g % 4 + 1) * 128], lhsT=qTs[:, g, :], rhs=keysT[:, g % 2, :], start=True, stop=True), [qTsb, keysTb], [psb])
                    if g % 4 == 3:
                        self.evac(sc[:, g - 3:g + 1, :], ps[:].rearrange("p (g q) -> p g q", g=4), [psb], [scb])
                m16bs = [Buf(f"m16_{g}") for g in range(16)]; wkbs = [Buf(f"wk_{g}") for g in range(16)]; ixbs = [Buf(f"ix_{g}") for g in range(16)]
                for g in range(16):
                    k.op("dve", lambda e, g=g: e.max(out=m16[:, g, 0:8], in_=sc[:, g, :]), [scb], [m16bs[g]])
                for g in range(16):
                    k.op("dve", lambda e, g=g: e.match_replace(out=wk[:, g, :], in_to_replace=m16[:, g, 0:8], in_values=sc[:, g, :], imm_value=-1e30), [scb, m16bs[g]], [wkbs[g], wkb])
                for g in range(16):
                    k.op("dve", lambda e, g=g: e.max(out=m16[:, g, 8:16], in_=wk[:, g, :]), [wkbs[g]], [m16bs[g]])
                for g in range(16):
                    k.op("dve", lambda e, g=g: e.max_index(out=ix[:, g, 0:8], in_max=m16[:, g, 0:8], in_values=sc[:, g, :]), [scb, m16bs[g]], [ixbs[g]])
                for g in range(16):
                    k.op("dve", lambda e, g=g: e.max_index(out=ix[:, g, 8:16], in_max=m16[:, g, 8:16], in_values=wk[:, g, :]), [wkbs[g], m16bs[g]], [ixbs[g], ixb, m16b])
                k.op("dve", lambda e: e.tensor_copy(out=ixf[:], in_=ix[:]), [ixb], [ixfb])
                m4 = m16[:].rearrange("p (h f) k -> p h f k", f=2)
                c4 = cand[:].rearrange("p h (i j) -> p h i j", i=16)
                k.op("dve", lambda e, m4=m4, c4=c4: e.tensor_tensor(out=c4, in0=m4[:, :, 0, :].unsqueeze(3).to_broadcast([128, 8, 16, 16]), in1=m4[:, :, 1, :].unsqueeze(2).to_broadcast([128, 8, 16, 16]), op=ALU.add), [m16b], [candb])
                cvbs = [Buf(f"cv_{h_}") for h_ in range(8)]; cwkbs = [Buf(f"cwk_{h_}") for h_ in range(8)]; cibs = [Buf(f"ci_{h_}") for h_ in range(8)]
                for hh in range(8):
                    k.op("dve", lambda e, hh=hh: e.max(out=cv[:, hh, 0:8], in_=cand[:, hh, :]), [candb], [cvbs[hh]])
                for hh in range(8):
                    k.op("dve", lambda e, hh=hh: e.match_replace(out=cwk[:, hh, :], in_to_replace=cv[:, hh, 0:8], in_values=cand[:, hh, :], imm_value=-1e30), [candb, cvbs[hh]], [cwkbs[hh], cwkb])
                for hh in range(8):
                    k.op("dve", lambda e, hh=hh: e.max(out=cv[:, hh, 8:16], in_=cwk[:, hh, :]), [cwkbs[hh]], [cvbs[hh]])
                for hh in range(8):
                    k.op("dve", lambda e, hh=hh: e.max_index(out=ci[:, hh, 0:8], in_max=cv[:, hh, 0:8], in_values=cand[:, hh, :]), [candb, cvbs[hh]], [cibs[hh]])
                for hh in range(8):
                    k.op("dve", lambda e, hh=hh: e.max_index(out=ci[:, hh, 8:16], in_max=cv[:, hh, 8:16], in_values=cwk[:, hh, :]), [cwkbs[hh], cvbs[hh]], [cibs[hh], cib, cvb])
                k.op("dve", lambda e: e.tensor_single_scalar(out=cj[:], in_=ci[:], scalar=15, op=ALU.bitwise_and), [cib], [cjb])
                k.op("dve", lambda e: e.tensor_single_scalar(out=ci[:], in_=ci[:], scalar=4, op=ALU.logical_shift_right), [cib], [cib])
                k.op("dve", lambda e: e.tensor_copy(out=cif[:], in_=ci[:]), [cib], [cifb])
                k.op("dve", lambda e: e.tensor_copy(out=cjf[:], in_=cj[:]), [cjb], [cjfb])
                x4 = ixf[:].rearrange("p (h f) k -> p h f k", f=2)
                for (cf, cfb, hf, dst, dstb) in ((cif, cifb, 0, i1, i1b), (cjf, cjfb, 1, i2, i2b)):
                    k.op("dve", lambda e, cf=cf: e.tensor_tensor(out=E, in0=cf[:].unsqueeze(3).to_broadcast([128, 8, 16, 16]), in1=iot[:].unsqueeze(1).unsqueeze(1).to_broadcast([128, 8, 16, 16]), op=ALU.is_equal), [cfb, iotb], [Eb])
                    k.op("dve", lambda e, hf=hf, x4=x4: e.tensor_tensor(out=E, in0=E, in1=x4[:, :, hf, :].unsqueeze(2).to_broadcast([128, 8, 16, 16]), op=ALU.mult), [Eb, ixfb], [Eb])
                    k.op("dve", lambda e, dst=dst: e.tensor_reduce(out=dst[:], in_=E, axis=AX.X, op=ALU.add), [Eb], [dstb])
                k.op("dve", lambda e: e.scalar_tensor_tensor(out=i1[:], in0=i1[:], scalar=128.0, in1=i2[:], op0=ALU.mult, op1=ALU.add), [i1b, i2b], [i1b])
                k.op("dve", lambda e: e.tensor_scalar(out=eid[:].rearrange("p (h k) -> p h k", h=8), in0=i1[:], scalar1=0.0, scalar2=None, op0=ALU.add), [i1b], [eidb])
                k.op("dve", lambda e: e.tensor_tensor(out=gw[:], in0=cv[:], in1=cv[:, :, 0:1].to_broadcast([128, 8, 16]), op=ALU.subtract), [cvb], [gwb])
                k.op("act", lambda e: e.activation(out=gw[:], in_=gw[:], func=AF.Exp), [gwb], [gwb])
                k.op("dve", lambda e: e.tensor_reduce(out=gs[:], in_=gw[:], axis=AX.X, op=ALU.add), [gwb], [gsb])
                k.op("dve", lambda e: e.reciprocal(out=gs[:], in_=gs[:]), [gsb], [gsb])
                k.op("dve", lambda e: e.tensor_tensor(out=gw[:], in0=gw[:], in1=gs[:].unsqueeze(2).to_broadcast([128, 8, 16]), op=ALU.mult), [gwb, gsb], [gwb])
                G = 4
                actbs = [Buf(f"act_{i_}") for i_ in range(128)]
                for s0 in range(0, 128, G):
                    grp = []
                    for sl_ in range(s0, s0 + G):
                        gb_, gbb_ = gbuf[ng % NG]; ng += 1
                        grp.append((sl_, gb_, gbb_))
                        jk_, jkb_ = junks[sl_ % 2]
                        k.dma("pool", lambda e, gb_=gb_, sl_=sl_: e.indirect_dma_start(out=gb_[:, :], out_offset=None, in_=self.uvb[:, :], in_offset=bass.IndirectOffsetOnAxis(ap=eid[:, sl_:sl_ + 1], axis=0)), [eidb, self.uvb_b], [gbb_])
                        k.op("dve", lambda e, gb_=gb_, sl_=sl_, jk_=jk_: e.scalar_tensor_tensor(out=jk_[:], in0=gb_[:, 0:D], scalar=1.0, in1=h2[:], op0=ALU.mult, op1=ALU.mult, accum_out=act[:, sl_:sl_ + 1]), [gbb_, h2b], [jkb_, actbs[sl_]])
                    k.op("act", lambda e, s0=s0: e.activation(out=wact[:, s0:s0 + G], in_=act[:, s0:s0 + G], func=AF.Gelu), actbs[s0:s0 + G], [wactb])
                    k.op("dve", lambda e, s0=s0: e.tensor_tensor(out=wact[:, s0:s0 + G], in0=wact[:, s0:s0 + G], in1=gw[:].rearrange("p h k -> p (h k)")[:, s0:s0 + G], op=ALU.mult), [wactb, gwb], [wactb])
                    for (sl_, gb_, gbb_) in grp:
                        ac_, acb_ = accs[sl_ % 2]
                        if sl_ < 2:
                            k.op("dve", lambda e, gb_=gb_, sl_=sl_, ac_=ac_: e.tensor_scalar(out=ac_[:], in0=gb_[:, D:2 * D], scalar1=wact[:, sl_:sl_ + 1], scalar2=None, op0=ALU.mult), [gbb_, wactb], [acb_])
                        else:
                            k.op("dve", lambda e, gb_=gb_, sl_=sl_, ac_=ac_: e.scalar_tensor_tensor(out=ac_[:], in0=gb_[:, D:2 * D], scalar=wact[:, sl_:sl_ + 1], in1=ac_[:], op0=ALU.mult, op1=ALU.add), [gbb_, wactb, acb_], [acb_])
                k.op("dve", lambda e: e.tensor_tensor(out=acc[:], in0=accs[0][0][:], in1=accs[1][0][:], op=ALU.add), [accs[0][1], accs[1][1]], [accb])
                k.op("pool", lambda e: e.tensor_tensor(out=acc[:], in0=acc[:], in1=gt2[:], op=ALU.mult), [accb, gt2b], [accb])
                k.op("pool", lambda e: e.tensor_tensor(out=xt[:], in0=xt[:], in1=acc[:], op=ALU.add), [xtb, accb], [xtb])
                k.dma("sp", lambda e, t0=t0: e.dma_start(out=self.xs[b, t0:t0 + 128, :], in_=xt[:]), [xtb], [self.xs_b])
        k.es = k.es0
        k.barrier()


def _rope_tables(S):
    rows = S // 64
    row = np.repeat(np.arange(rows), 64).astype(np.float32)
    col = np.tile(np.arange(64), rows).astype(np.float32)
    inv = np.power(np.float32(10000.0), -np.arange(16, dtype=np.float32) / 16).astype(np.float32)
    ar = row[:, None] * inv
    ac = col[:, None] * inv
    ang = np.concatenate([ar, ar, ac, ac], axis=-1).astype(np.float32)
    cos = np.cos(ang).astype(np.float32)
    sin = np.sin(ang).astype(np.float32).reshape(-1, 2, 2, 16).copy()
    sin[:, :, 0, :] *= -1.0
    return cos, sin.reshape(-1, 64)


def _const_masks():
    p = np.arange(128)[:, None]; f = np.arange(128)[None, :]
    same = (p // 64) == (f // 64)
    m = np.zeros((8, 128, 128), np.float32)
    m[0] = same & (p <= f); m[1] = same & (p >= f); m[2] = same & (p < f); m[3] = same & (p > f)
    m[4] = same; m[5] = (p < 64) & (f >= 0); m[6] = (p >= 64) & (f >= 0)
    return (p >= f).astype(np.float32), (p <= f).astype(np.float32), m


def kernel(**inputs):
    from concourse.bass_utils import run_bass_kernel_spmd
    x = np.asarray(inputs["x"], dtype=np.float32)
    B, S, _ = x.shape
    L = inputs["ctx"].shape[1]
    DEPTH = inputs["w_in"].shape[0]
    m = M(dict(NB=1, S=S, L=L, DEPTH=DEPTH))
    nc = m.build()
    shared = {k_: np.ascontiguousarray(np.asarray(v, dtype=np.float32)) for k_, v in inputs.items()
              if k_ not in ("x", "c", "ctx")}
    shared["peer_uv"] = np.ascontiguousarray(np.concatenate(
        [shared.pop("peer_u").reshape(-1, D), shared.pop("peer_v").reshape(-1, D)], axis=1))
    shared["ident"] = np.eye(128, dtype=np.float32)
    shared["rope_cos"], shared["rope_sin"] = _rope_tables(S)
    shared["mask_lo"], shared["mask_hi"], shared["gmasks"] = _const_masks()
    c = np.asarray(inputs["c"], dtype=np.float32)
    ctx = np.asarray(inputs["ctx"], dtype=np.float32)
    in_maps = []
    for b in range(B):
        im = dict(shared)
        im["x"] = np.ascontiguousarray(x[b:b + 1])
        im["c"] = np.ascontiguousarray(c[b:b + 1])
        im["ctx"] = np.ascontiguousarray(ctx[b:b + 1])
        in_maps.append(im)
    res = run_bass_kernel_spmd(nc, in_maps, core_ids=list(range(B)))
    return np.concatenate([np.asarray(r["out"], dtype=np.float32) for r in res.results], axis=0)
```
